# Optimizing a Trainium2 kernel written in Bass

```python
import jax, jax.numpy as jnp
from jax import lax
import numpy as np

D_MODEL = 2048
BATCH = 1
SEQ = 8192
DEPTH = 4

N_MIXERS = 4
N_HEADS = 16
HEAD_DIM = 128
MIX_WIDTH = N_HEADS * HEAD_DIM
Q_BLOCK = 128
EPS = 1e-6
NEG_BIG = -1e30

MLA_Q_RANK = 512
MLA_KV_RANK = 256
MLA_NOPE = 128
MLA_ROPE = 64
MLA_V = 128
MLA_QK_DIM = MLA_NOPE + MLA_ROPE
ROPE_THETA = 10000.0
MLA_IN = MLA_Q_RANK + MLA_KV_RANK + MLA_ROPE + MIX_WIDTH

MOBA_BLOCK = 256
MOBA_TOPK = 3
MOBA_Q_CHUNK = 16

QKV_GATE_IN = 4 * MIX_WIDTH

NSA_GROUPS = 4
NSA_HPG = N_HEADS // NSA_GROUPS
NSA_CMP_BLOCK = 32
NSA_CMP_STRIDE = 16
NSA_SEL_BLOCK = 64
NSA_SEL_N = 16
NSA_WINDOW = 512
NSA_Q_CHUNK = 32
NSA_KV_WIDTH = 2 * NSA_GROUPS * HEAD_DIM
NSA_IN = MIX_WIDTH + 3 * NSA_KV_WIDTH + 3 * N_HEADS + MIX_WIDTH

kernel_name = 'hybrid_mla_moba_stickbreak_nsa'


def layers_of(m):
    return len(range(m, DEPTH, N_MIXERS))


def rms_norm(x, g):
    xf = x.astype(jnp.float32)
    y = xf * lax.rsqrt(jnp.mean(xf * xf, axis=-1, keepdims=True) + EPS)
    return (y * g.astype(jnp.float32)).astype(x.dtype)


def alibi_slopes():
    return jnp.asarray(2.0 ** (-8.0 * np.arange(1, N_HEADS + 1) / N_HEADS), dtype=jnp.float32)


def to_heads(t, n, d):
    B, S, _ = t.shape
    return t.reshape(B, S, n, d).transpose(0, 2, 1, 3)


def from_heads(t):
    B, n, S, d = t.shape
    return t.transpose(0, 2, 1, 3).reshape(B, S, n * d)


def rope(x):
    S, R = x.shape[1], x.shape[-1]
    inv_freq = ROPE_THETA ** (-jnp.arange(0, R, 2, dtype=jnp.float32) / R)
    ang = jnp.arange(S, dtype=jnp.float32)[:, None] * inv_freq[None, :]
    cos = jnp.cos(ang)[None, :, None, :]
    sin = jnp.sin(ang)[None, :, None, :]
    x1, x2 = jnp.split(x.astype(jnp.float32), 2, axis=-1)
    return jnp.concatenate([x1 * cos - x2 * sin, x1 * sin + x2 * cos], axis=-1).astype(x.dtype)


def causal_block_attention(q, k, v, scale):
    B, H, S, dk = q.shape
    nq = S // Q_BLOCK
    qb = q.reshape(B, H, nq, Q_BLOCK, dk).transpose(2, 0, 1, 3, 4)
    k_pos = jnp.arange(S)

    def one(args):
        qi, i = args
        q_pos = i * Q_BLOCK + jnp.arange(Q_BLOCK)
        s = jnp.einsum('bhqd,bhkd->bhqk', qi, k).astype(jnp.float32) * scale
        s = jnp.where(k_pos[None, :] <= q_pos[:, None], s, -jnp.inf)
        p = jax.nn.softmax(s, axis=-1)
        return jnp.einsum('bhqk,bhkd->bhqd', p.astype(v.dtype), v)

    o = lax.map(one, (qb, jnp.arange(nq)))
    return o.transpose(1, 2, 0, 3, 4).reshape(B, H, S, v.shape[-1])


def mla_mixer(h, w_in, q_lat_norm, w_q_up, kv_lat_norm, w_kv_up, q_norm, k_norm):
    B, S, _ = h.shape
    proj = h @ w_in
    c1 = MLA_Q_RANK
    c2 = c1 + MLA_KV_RANK
    c3 = c2 + MLA_ROPE
    q_lat, kv_lat, k_rope, gate = proj[..., :c1], proj[..., c1:c2], proj[..., c2:c3], proj[..., c3:]
    q = (rms_norm(q_lat, q_lat_norm) @ w_q_up).reshape(B, S, N_HEADS, MLA_QK_DIM)
    kv = (rms_norm(kv_lat, kv_lat_norm) @ w_kv_up).reshape(B, S, N_HEADS, MLA_NOPE + MLA_V)
    k_nope, v = kv[..., :MLA_NOPE], kv[..., MLA_NOPE:]
    q = jnp.concatenate([rms_norm(q[..., :MLA_NOPE], q_norm[:MLA_NOPE]),
                         rope(rms_norm(q[..., MLA_NOPE:], q_norm[MLA_NOPE:]))], axis=-1)
    k_nope = rms_norm(k_nope, k_norm[:MLA_NOPE])
    k_rope = rope(rms_norm(k_rope, k_norm[MLA_NOPE:])[:, :, None, :])
    k = jnp.concatenate([k_nope, jnp.broadcast_to(k_rope, (B, S, N_HEADS, MLA_ROPE))], axis=-1)
    o = causal_block_attention(q.transpose(0, 2, 1, 3), k.transpose(0, 2, 1, 3),
                               v.transpose(0, 2, 1, 3), MLA_QK_DIM ** -0.5)
    return from_heads(o) * jax.nn.silu(gate)


def moba_attention(q, k, v, slopes):
    B, H, S, D = q.shape
    Sp = -(-S // MOBA_BLOCK) * MOBA_BLOCK
    pad = ((0, 0), (0, 0), (0, Sp - S), (0, 0))
    q, k, v = jnp.pad(q, pad), jnp.pad(k, pad), jnp.pad(v, pad)
    NB = Sp // MOBA_BLOCK
    kb = k.reshape(B, H, NB, MOBA_BLOCK, D)
    vb = v.reshape(B, H, NB, MOBA_BLOCK, D)
    k_mean = jnp.mean(kb.astype(jnp.float32), axis=3)
    score = jnp.einsum('bhsd,bhnd->bhsn', q.astype(jnp.float32), k_mean)
    q_blk = jnp.arange(Sp) // MOBA_BLOCK
    past = jnp.arange(NB)[None, :] < q_blk[:, None]
    score = jnp.where(past, score, -jnp.inf)
    kk = min(MOBA_TOPK, NB)
    top_val, top_idx = lax.top_k(score, kk)
    top_ok = top_val > -jnp.inf

    Cq = MOBA_Q_CHUNK
    nc = Sp // Cq
    qc = q.reshape(B, H, nc, Cq, D).transpose(2, 0, 1, 3, 4)
    ic = top_idx.reshape(B, H, nc, Cq, kk).transpose(2, 0, 1, 3, 4)
    okc = top_ok.reshape(B, H, nc, Cq, kk).transpose(2, 0, 1, 3, 4)
    bi = jnp.arange(B)[:, None, None, None]
    hi = jnp.arange(H)[None, :, None, None]
    sl = slopes[None, :, None, None]
    scale = D ** -0.5
    offs = jnp.arange(MOBA_BLOCK)

    def one(args):
        qi, idx, ok, c = args
        q_pos = c * Cq + jnp.arange(Cq)
        own = (c * Cq) // MOBA_BLOCK
        k_sel = kb[bi, hi, idx].reshape(B, H, Cq, kk * MOBA_BLOCK, D)
        v_sel = vb[bi, hi, idx].reshape(B, H, Cq, kk * MOBA_BLOCK, D)
        sel_pos = (idx[..., None] * MOBA_BLOCK + offs).reshape(B, H, Cq, kk * MOBA_BLOCK)
        sel_ok = jnp.repeat(ok, MOBA_BLOCK, axis=-1)
        d_sel = (q_pos[:, None] - sel_pos).astype(jnp.float32)
        s_sel = jnp.einsum('bhqd,bhqkd->bhqk', qi, k_sel).astype(jnp.float32) * scale - sl * d_sel
        s_sel = jnp.where(sel_ok, s_sel, -jnp.inf)
        k_own = lax.dynamic_index_in_dim(kb, own, axis=2, keepdims=False)
        v_own = lax.dynamic_index_in_dim(vb, own, axis=2, keepdims=False)
        d_own = q_pos[:, None] - (own * MOBA_BLOCK + offs)[None, :]
        s_own = jnp.einsum('bhqd,bhkd->bhqk', qi, k_own).astype(jnp.float32) * scale - sl * d_own.astype(jnp.float32)
        s_own = jnp.where(d_own >= 0, s_own, -jnp.inf)
        p = jax.nn.softmax(jnp.concatenate([s_sel, s_own], axis=-1), axis=-1).astype(v.dtype)
        n_sel = kk * MOBA_BLOCK
        return (jnp.einsum('bhqk,bhqkd->bhqd', p[..., :n_sel], v_sel)
                + jnp.einsum('bhqk,bhkd->bhqd', p[..., n_sel:], v_own))

    o = lax.map(one, (qc, ic, okc, jnp.arange(nc)))
    return o.transpose(1, 2, 0, 3, 4).reshape(B, H, Sp, D)[:, :, :S]


def moba_mixer(h, w_in, q_norm, k_norm):
    q, k, v, gate = jnp.split(h @ w_in, 4, axis=-1)
    q = rms_norm(to_heads(q, N_HEADS, HEAD_DIM), q_norm)
    k = rms_norm(to_heads(k, N_HEADS, HEAD_DIM), k_norm)
    v = to_heads(v, N_HEADS, HEAD_DIM)
    o = moba_attention(q, k, v, alibi_slopes())
    return from_heads(o) * jax.nn.silu(gate)


def stick_breaking_attention(q, k, v):
    B, H, S, D = q.shape
    nq = S // Q_BLOCK
    qb = q.reshape(B, H, nq, Q_BLOCK, D).transpose(2, 0, 1, 3, 4)
    k_pos = jnp.arange(S)
    scale = D ** -0.5

    def one(args):
        qi, i = args
        q_pos = i * Q_BLOCK + jnp.arange(Q_BLOCK)
        before = k_pos[None, :] < q_pos[:, None]
        z = jnp.einsum('bhqd,bhkd->bhqk', qi, k).astype(jnp.float32) * scale
        log_keep = jnp.where(before, jax.nn.log_sigmoid(-z), 0.0)
        log_between = lax.cumsum(log_keep, axis=3, reverse=True) - log_keep
        a = jnp.where(before, jnp.exp(jax.nn.log_sigmoid(z) + log_between), 0.0)
        return jnp.einsum('bhqk,bhkd->bhqd', a.astype(v.dtype), v)

    o = lax.map(one, (qb, jnp.arange(nq)))
    return o.transpose(1, 2, 0, 3, 4).reshape(B, H, S, D)


def stick_breaking_mixer(h, w_in):
    q, k, v, gate = jnp.split(h @ w_in, 4, axis=-1)
    o = stick_breaking_attention(to_heads(q, N_HEADS, HEAD_DIM), to_heads(k, N_HEADS, HEAD_DIM),
                                 to_heads(v, N_HEADS, HEAD_DIM))
    return from_heads(o) * jax.nn.silu(gate)


def nsa_compress(t, w, pos):
    S = t.shape[2]
    n_cmp = (S - NSA_CMP_BLOCK) // NSA_CMP_STRIDE + 1
    idx = np.arange(n_cmp)[:, None] * NSA_CMP_STRIDE + np.arange(NSA_CMP_BLOCK)[None, :]
    blocks = t[:, :, idx] + pos
    return blocks.reshape(blocks.shape[0], blocks.shape[1], n_cmp, -1) @ w


def nsa_compressed_branch(q, k_cmp, v_cmp, slopes):
    B, G, P, S, D = q.shape
    n_cmp = k_cmp.shape[2]
    n_sel = S // NSA_SEL_BLOCK
    kn = min(NSA_SEL_N, n_sel)
    cmp_end = jnp.arange(n_cmp) * NSA_CMP_STRIDE + NSA_CMP_BLOCK - 1
    c_start = np.arange(n_cmp)[:, None] * NSA_CMP_STRIDE
    s_start = np.arange(n_sel)[None, :] * NSA_SEL_BLOCK
    cmp_to_sel = jnp.asarray((c_start < s_start + NSA_SEL_BLOCK) & (c_start + NSA_CMP_BLOCK > s_start),
                             dtype=jnp.float32)
    sl = slopes[None, :, :, None, None]
    scale = D ** -0.5
    nq = S // Q_BLOCK
    qb = q.reshape(B, G, P, nq, Q_BLOCK, D).transpose(3, 0, 1, 2, 4, 5)
    blk_ids = jnp.arange(n_sel)

    def one(args):
        qi, i = args
        q_pos = i * Q_BLOCK + jnp.arange(Q_BLOCK)
        dist = q_pos[:, None] - cmp_end[None, :]
        mask = dist >= 0
        s = jnp.einsum('bgpqd,bgnd->bgpqn', qi, k_cmp).astype(jnp.float32) * scale - sl * dist.astype(jnp.float32)
        p = jax.nn.softmax(jnp.where(mask, s, NEG_BIG), axis=-1) * mask
        o = jnp.einsum('bgpqn,bgnd->bgpqd', p.astype(v_cmp.dtype), v_cmp)
        imp = jnp.einsum('bgpqn,nj->bgqj', p, cmp_to_sel)
        cur = (q_pos // NSA_SEL_BLOCK)[:, None]
        forced = (blk_ids == 0) | (blk_ids == cur) | (blk_ids == cur - 1)
        imp = jnp.where(blk_ids > cur, -jnp.inf, jnp.where(forced, jnp.inf, imp))
        val, idx = lax.top_k(imp, kn)
        return o, idx, val > -jnp.inf

    o, idx, ok = lax.map(one, (qb, jnp.arange(nq)))
    o = o.transpose(1, 2, 3, 0, 4, 5).reshape(B, G, P, S, D)
    idx = idx.transpose(1, 2, 0, 3, 4).reshape(B, G, S, kn)
    ok = ok.transpose(1, 2, 0, 3, 4).reshape(B, G, S, kn)
    return o, idx, ok


def nsa_selected_branch(q, k, v, sel_idx, sel_ok, slopes):
    B, G, P, S, D = q.shape
    n_sel = S // NSA_SEL_BLOCK
    kn = sel_idx.shape[-1]
    kb = k.reshape(B, G, n_sel, NSA_SEL_BLOCK, D)
    vb = v.reshape(B, G, n_sel, NSA_SEL_BLOCK, D)
    Cq = NSA_Q_CHUNK
    nc = S // Cq
    qc = q.reshape(B, G, P, nc, Cq, D).transpose(3, 0, 1, 2, 4, 5)
    ic = sel_idx.reshape(B, G, nc, Cq, kn).transpose(2, 0, 1, 3, 4)
    okc = sel_ok.reshape(B, G, nc, Cq, kn).transpose(2, 0, 1, 3, 4)
    bi = jnp.arange(B)[:, None, None, None]
    gi = jnp.arange(G)[None, :, None, None]
    sl = slopes[None, :, :, None, None]
    scale = D ** -0.5
    offs = jnp.arange(NSA_SEL_BLOCK)
    n_keys = kn * NSA_SEL_BLOCK

    def one(args):
        qi, idx, ok, c = args
        q_pos = c * Cq + jnp.arange(Cq)
        k_sel = kb[bi, gi, idx].reshape(B, G, Cq, n_keys, D)
        v_sel = vb[bi, gi, idx].reshape(B, G, Cq, n_keys, D)
        k_pos = (idx[..., None] * NSA_SEL_BLOCK + offs).reshape(B, G, Cq, n_keys)
        dist = q_pos[:, None] - k_pos
        mask = (dist >= 0) & jnp.repeat(ok, NSA_SEL_BLOCK, axis=-1)
        s = jnp.einsum('bgpqd,bgqkd->bgpqk', qi, k_sel).astype(jnp.float32) * scale - sl * dist[:, :, None].astype(jnp.float32)
        p = jax.nn.softmax(jnp.where(mask[:, :, None], s, -jnp.inf), axis=-1)
        return jnp.einsum('bgpqk,bgqkd->bgpqd', p.astype(v.dtype), v_sel)

    o = lax.map(one, (qc, ic, okc, jnp.arange(nc)))
    return o.transpose(1, 2, 3, 0, 4, 5).reshape(B, G, P, S, D)


def nsa_window_branch(q, k, v, slopes):
    B, G, P, S, D = q.shape
    W = NSA_WINDOW
    L = W + Q_BLOCK
    pad = ((0, 0), (0, 0), (W, 0), (0, 0))
    kp, vp = jnp.pad(k, pad), jnp.pad(v, pad)
    sl = slopes[None, :, :, None, None]
    scale = D ** -0.5
    nq = S // Q_BLOCK
    qb = q.reshape(B, G, P, nq, Q_BLOCK, D).transpose(3, 0, 1, 2, 4, 5)

    def one(args):
        qi, i = args
        qs = i * Q_BLOCK
        kw = lax.dynamic_slice_in_dim(kp, qs, L, axis=2)
        vw = lax.dynamic_slice_in_dim(vp, qs, L, axis=2)
        k_pos = qs - W + jnp.arange(L)
        q_pos = qs + jnp.arange(Q_BLOCK)
        dist = q_pos[:, None] - k_pos[None, :]
        mask = (dist >= 0) & (dist < W) & (k_pos[None, :] >= 0)
        s = jnp.einsum('bgpqd,bgkd->bgpqk', qi, kw).astype(jnp.float32) * scale - sl * dist.astype(jnp.float32)
        p = jax.nn.softmax(jnp.where(mask, s, -jnp.inf), axis=-1)
        return jnp.einsum('bgpqk,bgkd->bgpqd', p.astype(v.dtype), vw)

    o = lax.map(one, (qb, jnp.arange(nq)))
    return o.transpose(1, 2, 3, 0, 4, 5).reshape(B, G, P, S, D)


def nsa_mixer(h, w_in, q_norm, k_norm, w_cmp_k, w_cmp_v, cmp_pos):
    B, S, _ = h.shape
    G, P, D = NSA_GROUPS, NSA_HPG, HEAD_DIM
    cuts = np.cumsum([MIX_WIDTH, NSA_KV_WIDTH, NSA_KV_WIDTH, NSA_KV_WIDTH, 3 * N_HEADS]).tolist()
    q, kv_c, kv_s, kv_w, g_br, gate = jnp.split(h @ w_in, cuts, axis=-1)
    q = rms_norm(to_heads(q, N_HEADS, D), q_norm).reshape(B, G, P, S, D)

    def kv_split(t):
        kk, vv = jnp.split(t, 2, axis=-1)
        return to_heads(kk, G, D), to_heads(vv, G, D)

    kc, vc = kv_split(kv_c)
    k_cmp = rms_norm(nsa_compress(kc, w_cmp_k, cmp_pos), k_norm[0])
    v_cmp = nsa_compress(vc, w_cmp_v, cmp_pos)
    ks, vs = kv_split(kv_s)
    ks = rms_norm(ks, k_norm[1])
    kw, vw = kv_split(kv_w)
    kw = rms_norm(kw, k_norm[2])
    slopes = alibi_slopes().reshape(G, P)
    o_cmp, sel_idx, sel_ok = nsa_compressed_branch(q, k_cmp, v_cmp, slopes)
    o_slc = nsa_selected_branch(q, ks, vs, sel_idx, sel_ok, slopes)
    o_win = nsa_window_branch(q, kw, vw, slopes)
    g = jax.nn.sigmoid(g_br.astype(jnp.float32)).reshape(B, S, G, P, 3).transpose(4, 0, 2, 3, 1)[..., None]
    o = (g[0] * o_cmp + g[1] * o_slc + g[2] * o_win).astype(h.dtype)
    return from_heads(o.reshape(B, N_HEADS, S, D)) * jax.nn.silu(gate)


def setup_inputs(seed: int = 0) -> dict:
    key = jax.random.key(seed)
    keys = iter(jax.random.split(key, 32))

    def dense(shape, fan_in):
        return jax.random.normal(next(keys), shape, jnp.float32) * (fan_in ** -0.5)

    def gain(shape):
        return 1.0 + 0.02 * jax.random.normal(next(keys), shape, jnp.float32)

    na, nb, nc, nd = (layers_of(m) for m in range(N_MIXERS))
    D = D_MODEL
    cmp_in = NSA_CMP_BLOCK * HEAD_DIM
    return {
        'x': jax.random.normal(next(keys), (BATCH, SEQ, D), jnp.float32),
        'norm_a': gain((na, D)),
        'w_in_a': dense((na, D, MLA_IN), D),
        'q_lat_norm_a': gain((na, MLA_Q_RANK)),
        'w_q_up_a': dense((na, MLA_Q_RANK, N_HEADS * MLA_QK_DIM), MLA_Q_RANK),
        'kv_lat_norm_a': gain((na, MLA_KV_RANK)),
        'w_kv_up_a': dense((na, MLA_KV_RANK, N_HEADS * (MLA_NOPE + MLA_V)), MLA_KV_RANK),
        'q_norm_a': gain((na, MLA_QK_DIM)),
        'k_norm_a': gain((na, MLA_QK_DIM)),
        'w_out_a': dense((na, MIX_WIDTH, D), MIX_WIDTH),
        'norm_b': gain((nb, D)),
        'w_in_b': dense((nb, D, QKV_GATE_IN), D),
        'q_norm_b': gain((nb, HEAD_DIM)),
        'k_norm_b': gain((nb, HEAD_DIM)),
        'w_out_b': dense((nb, MIX_WIDTH, D), MIX_WIDTH),
        'norm_c': gain((nc, D)),
        'w_in_c': dense((nc, D, QKV_GATE_IN), D),
        'w_out_c': dense((nc, MIX_WIDTH, D), MIX_WIDTH),
        'norm_d': gain((nd, D)),
        'w_in_d': dense((nd, D, NSA_IN), D),
        'q_norm_d': gain((nd, HEAD_DIM)),
        'k_norm_d': gain((nd, 3, HEAD_DIM)),
        'w_cmp_k_d': dense((nd, cmp_in, HEAD_DIM), cmp_in),
        'w_cmp_v_d': dense((nd, cmp_in, HEAD_DIM), cmp_in),
        'cmp_pos_d': 0.1 * jax.random.normal(next(keys), (nd, NSA_CMP_BLOCK, HEAD_DIM), jnp.float32),
        'w_out_d': dense((nd, MIX_WIDTH, D), MIX_WIDTH),
    }


def reference(x, norm_a, w_in_a, q_lat_norm_a, w_q_up_a, kv_lat_norm_a, w_kv_up_a, q_norm_a, k_norm_a, w_out_a,
              norm_b, w_in_b, q_norm_b, k_norm_b, w_out_b,
              norm_c, w_in_c, w_out_c,
              norm_d, w_in_d, q_norm_d, k_norm_d, w_cmp_k_d, w_cmp_v_d, cmp_pos_d, w_out_d):
    for i in range(DEPTH):
        m, j = i % N_MIXERS, i // N_MIXERS
        if m == 0:
            y = mla_mixer(rms_norm(x, norm_a[j]), w_in_a[j], q_lat_norm_a[j], w_q_up_a[j],
                          kv_lat_norm_a[j], w_kv_up_a[j], q_norm_a[j], k_norm_a[j]) @ w_out_a[j]
        elif m == 1:
            y = moba_mixer(rms_norm(x, norm_b[j]), w_in_b[j], q_norm_b[j], k_norm_b[j]) @ w_out_b[j]
        elif m == 2:
            y = stick_breaking_mixer(rms_norm(x, norm_c[j]), w_in_c[j]) @ w_out_c[j]
        else:
            y = nsa_mixer(rms_norm(x, norm_d[j]), w_in_d[j], q_norm_d[j], k_norm_d[j],
                          w_cmp_k_d[j], w_cmp_v_d[j], cmp_pos_d[j]) @ w_out_d[j]
        x = x + y
    return x
```

```python
import math
from contextlib import ExitStack

import numpy as np
import ml_dtypes

import concourse.bass as bass
import concourse.mybir as mybir
from concourse.bass_utils import run_bass_kernel_spmd

F32 = mybir.dt.float32
BF16 = mybir.dt.bfloat16
AF = mybir.ActivationFunctionType
ALU = mybir.AluOpType
NPBF = ml_dtypes.bfloat16

S = 8192
D = 2048
NCORE = 8
TOK = S // NCORE
EPS = 1e-6
NEG = -30000.0


class Sem:
    def __init__(self, h):
        self.h = h
        self.n = 0


class Buf:
    def __init__(self, name=""):
        self.name = name
        self.w = None
        self.r = {}


class Prog:
    ENGS = ("pe", "act", "dve", "pool", "sp")
    HAND = {"pe": "tensor", "act": "scalar", "dve": "vector", "pool": "gpsimd", "sp": "sync"}

    def __init__(self, nc, es):
        self.nc = nc
        self.es = es
        self.ops = {e: [] for e in self.ENGS}
        self.esem = {e: Sem(es.enter_context(nc.semaphore("s_" + e))) for e in self.ENGS}
        self.seen = {e: {} for e in self.ENGS}
        self.nsem = 0
        self.ntile = 0
        self.outsigs = []
        self.es_g = es
        self.all_sems = []
        self.free_sems = []
        self.phase_sems = []

    def sem(self, kind="sp"):
        fl = [x for x in self.free_sems if x.kind == kind]
        if fl:
            sm = fl[-1]
            self.free_sems.remove(sm)
        else:
            self.nsem += 1
            sm = Sem(self.es_g.enter_context(self.nc.semaphore("u%d" % self.nsem)))
            sm.kind = kind
            self.all_sems.append(sm)
        self.phase_sems.append(sm)
        return sm

    def barrier(self, release_sems=True):
        waits = [(sm, sm.n) for sm in self.all_sems if sm.n > 0]
        waits += [(self.esem[e], self.esem[e].n) for e in self.ENGS if e != "sp" and self.esem[e].n > 0]
        s = self.esem["sp"]
        s.n += 1
        self.ops["sp"].append((waits, lambda eng: eng.nop(), (s.h, 1)))
        for k, v in [(id(sm), n) for sm, n in waits]:
            self.seen["sp"][k] = v
        for e in self.ENGS:
            if e == "sp":
                continue
            se = self.esem[e]
            se.n += 1
            self.ops[e].append(([(s, s.n)], lambda eng: eng.nop(), (se.h, 1)))
            self.seen[e][id(s)] = s.n
        if release_sems:
            self.free_sems.extend(self.phase_sems)
            self.phase_sems = []

    def sb(self, shape, dt, name=None):
        self.ntile += 1
        return self.es.enter_context(self.nc.sbuf_tensor(name or ("t%d" % self.ntile), list(shape), dt))

    def ps(self, shape, dt=F32, name=None):
        self.ntile += 1
        return self.es.enter_context(self.nc.psum_tensor(name or ("p%d" % self.ntile), list(shape), dt))

    def op(self, eng, fn, reads=(), writes=(), dma_sem=None):
        waits = {}

        def need(dep):
            s, v, e = dep
            if eng == "pe" and e == "pe":
                return
            k = id(s)
            if self.seen[eng].get(k, 0) >= v:
                return
            if k not in waits or waits[k][1] < v:
                waits[k] = (s, v)

        for b in reads:
            if b.w is not None:
                need(b.w)
        for b in writes:
            if b.w is not None and not (dma_sem is not None and b.w[0] is dma_sem):
                need(b.w)
            for d in b.r.values():
                need(d)
        for k, (s, v) in waits.items():
            self.seen[eng][k] = v
        if dma_sem is not None:
            dma_sem.n += 16
            sig = (dma_sem, dma_sem.n, "dma")
            inc = (dma_sem.h, 16)
        else:
            s = self.esem[eng]
            s.n += 1
            sig = (s, s.n, eng)
            inc = (s.h, 1)
        self.ops[eng].append((list(waits.values()), fn, inc))
        for b in reads:
            k = id(sig[0])
            b.r[k] = sig
        for b in writes:
            b.w = sig
            b.r = {}
        return sig

    def mm(self, out, lhsT, rhs, start, stop, reads, writes, sgc=False):
        if sgc:
            return self.op("pe", lambda e: e.matmul(out, lhsT, rhs, start=start, stop=stop, skip_group_check=True),
                           reads, writes)
        return self.op("pe", lambda e: e.matmul(out, lhsT, rhs, start=start, stop=stop), reads, writes)

    def tr(self, out, in_, ident, reads, writes):
        return self.op("pe", lambda e: e.transpose(out, in_, ident), reads, writes)

    def act(self, out, in_, func, reads, writes, bias=None, scale=None, eng="act"):
        kw = {}
        if bias is not None:
            kw["bias"] = bias
        if scale is not None:
            kw["scale"] = scale
        return self.op(eng, lambda e: e.activation(out=out, in_=in_, func=func, **kw), reads, writes)

    def copy(self, eng, out, in_, reads, writes):
        if eng == "act":
            return self.op("act", lambda e: e.copy(out, in_), reads, writes)
        return self.op(eng, lambda e: e.tensor_copy(out, in_), reads, writes)

    def tt(self, eng, out, in0, in1, op, reads, writes):
        return self.op(eng, lambda e: e.tensor_tensor(out, in0, in1, op), reads, writes)

    def ts(self, eng, out, in0, s1, s2, op0, op1, reads, writes):
        if s2 is None:
            return self.op(eng, lambda e: e.tensor_scalar(out, in0, s1, None, op0), reads, writes)
        return self.op(eng, lambda e: e.tensor_scalar(out, in0, s1, s2, op0, op1), reads, writes)

    def stt(self, eng, out, in0, scalar, in1, op0, op1, reads, writes):
        return self.op(eng, lambda e: e.scalar_tensor_tensor(out, in0, scalar, in1, op0, op1), reads, writes)

    def recip(self, out, in_, reads, writes):
        return self.op("dve", lambda e: e.reciprocal(out, in_), reads, writes)

    def memset(self, eng, ap, val, writes):
        return self.op(eng, lambda e: e.memset(ap, val), (), writes)

    def dma(self, q, out, in_, reads, writes, sem, is_output=False):
        sig = self.op(q, lambda e: e.dma_start(out=out, in_=in_), reads, writes, dma_sem=sem)
        if is_output:
            self.outsigs.append(sig)
        return sig

    def emit(self):
        nc = self.nc
        fin = {}
        for s, v, _ in self.outsigs:
            if id(s) not in fin or fin[id(s)][1] < v:
                fin[id(s)] = (s, v)
        with nc.Block() as block:
            for e in self.ENGS:
                ops = self.ops[e]

                def body(engh, ops=ops, e=e):
                    for waits, fn, inc in ops:
                        for s, v in waits:
                            engh.wait_ge(s.h, v)
                        ins = fn(engh)
                        ins.then_inc(inc[0], inc[1])
                    if e == "sp":
                        for s, v in fin.values():
                            engh.wait_ge(s.h, v)
                getattr(block, self.HAND[e])(body)


def new_nc():
    return bass.Bass("TRN2", target_bir_lowering=False)


def build_B(with_proj, with_norm):
    nc = new_nc()
    xT = nc.dram_tensor("xT", [D, TOK], F32, kind="ExternalInput").ap()
    if with_proj:
        ogT = nc.dram_tensor("ogT", [D, TOK], BF16, kind="ExternalInput").ap()
        w = nc.dram_tensor("w", [D, D], F32, kind="ExternalInput").ap()
        xo = nc.dram_tensor("xo", [D, TOK], F32, kind="ExternalOutput").ap()
    if with_norm:
        g = nc.dram_tensor("g", [128, 16], F32, kind="ExternalInput").ap()
        hT = nc.dram_tensor("hT", [D, TOK], BF16, kind="ExternalOutput").ap()
    with ExitStack() as es:
        P = Prog(nc, es)
        xs = P.sb([128, 16, TOK], F32)
        bxs = [Buf() for _ in range(16)]
        xT_v = xT.rearrange("(c p) t -> p c t", p=128)
        sx = [P.sem() for _ in range(16)]
        for c in range(16):
            P.dma("sp", xs[:, c, :], xT_v[:, c, :], (), [bxs[c]], sx[c])
        ones = P.sb([128, 128], BF16)
        bones = Buf()
        P.memset("pool", ones[:], 1.0, [bones])
        if with_proj:
            og = P.sb([128, 16, TOK], BF16)
            bog = Buf()
            ogT_v = ogT.rearrange("(c p) t -> p c t", p=128)
            so = P.sem()
            for c4 in range(4):
                P.dma("sp", og[:, 4 * c4:4 * c4 + 4, :], ogT_v[:, 4 * c4:4 * c4 + 4, :], (), [bog], so)
            wst = [P.sb([128, 16, 128], F32) for _ in range(2)]
            bwst = [Buf() for _ in range(2)]
            swst = [P.sem() for _ in range(2)]
            wbf = [P.sb([128, 16, 128], BF16) for _ in range(2)]
            bwbf = [Buf() for _ in range(2)]
            w_v = w.rearrange("(c p) n -> p c n", p=128)
            psy = [P.ps([128, 512]) for _ in range(4)]
            bpsy = [Buf() for _ in range(4)]
            sxo = [P.sem("pool") for _ in range(2)]
        if with_norm:
            sq = [P.sb([128, TOK], BF16) for _ in range(2)]
            bsq = [Buf() for _ in range(2)]
            pss = [P.ps([128, 512]) for _ in range(2)]
            bpss = [Buf() for _ in range(2)]
        for m in range(16):
            if with_proj:
                sl = m % 2
                P.dma("sp", wst[sl][:], w_v[:, :, m * 128:(m + 1) * 128], (), [bwst[sl]], swst[sl])
                P.copy(("dve", "pool")[m % 2], wbf[sl][:], wst[sl][:], [bwst[sl]], [bwbf[sl]])
                for hf in range(2):
                    pb = (m % 2) * 2 + hf
                    for c in range(16):
                        P.mm(psy[pb][:], wbf[sl][:, c, :], og[:, c, hf * 512:(hf + 1) * 512],
                             c == 0, c == 15, [bwbf[sl], bog], [bpsy[pb]])
                    P.tt("dve", xs[:, m, hf * 512:(hf + 1) * 512], xs[:, m, hf * 512:(hf + 1) * 512],
                         psy[pb][:], ALU.add, [bpsy[pb], bxs[m]], [bxs[m]])
                P.dma("pool", xo[m * 128:(m + 1) * 128, :], xs[:, m, :], [bxs[m]], (), sxo[m % 2], is_output=True)
            if with_norm:
                sl = m % 2
                P.act(sq[sl][:], xs[:, m, :], AF.Square, [bxs[m]], [bsq[sl]])
                for hf in range(2):
                    P.mm(pss[hf][:], ones[:], sq[sl][:, hf * 512:(hf + 1) * 512], m == 0, m == 15,
                         [bones, bsq[sl]], [bpss[hf]])
        if with_norm:
            gs = P.sb([128, 16], F32)
            gp = P.sb([128, 16], F32)
            bgs, bgp = Buf(), Buf()
            sg = P.sem()
            P.dma("sp", gs[:], g, (), [bgs], sg)
            P.ts("dve", gp[:], gs[:], math.sqrt(D), None, ALU.mult, None, [bgs], [bgp])
            rt = P.sb([128, TOK], F32)
            rstd = P.sb([128, TOK], F32)
            brt, brstd = Buf(), Buf()
            for hf in range(2):
                P.act(rt[:, hf * 512:(hf + 1) * 512], pss[hf][:], AF.Sqrt, [bpss[hf]], [brt], bias=D * EPS, scale=1.0)
            hst = [P.sb([128, TOK], BF16) for _ in range(2)]
            bhst = [Buf() for _ in range(2)]
            sh = [P.sem() for _ in range(2)]
            P.recip(rstd[:], rt[:], [brt], [brstd])
            for m in range(16):
                sl = m % 2
                P.stt("dve", hst[sl][:], xs[:, m, :], gp[:, m:m + 1], rstd[:], ALU.mult, ALU.mult,
                      [bxs[m], bgp, brstd], [bhst[sl]])
                P.dma("sp", hT[m * 128:(m + 1) * 128, :], hst[sl][:], [bhst[sl]], (), sh[sl], is_output=True)
        P.emit()
    return nc


class ACtx:
    def __init__(self, P, nc, consts, bf16_bank=False):
        self.P = P
        self.nc = nc
        self.ssbank = 7
        self.ones = P.sb([128, 128], BF16)
        self.bones = Buf()
        P.memset("pool", self.ones[:], 1.0, [self.bones])
        self.ident = P.sb([128, 128], BF16)
        self.negtri = P.sb([128, 128], BF16)
        self.bconst = Buf()
        s = P.sem()
        P.dma("sp", self.ident[:], consts["ident"], (), [self.bconst], s)
        P.dma("sp", self.negtri[:], consts["negtri"], (), [self.bconst], s)
        nb = 7 if bf16_bank else 8
        self.psb = [P.ps([128, 512]) for _ in range(nb)]
        self.bps = [Buf() for _ in range(nb)]
        if bf16_bank:
            self.ssbank = 6
            self.pst = P.ps([128, 1024], BF16)
            self.bpst = [Buf() for _ in range(8)]
        self.sq = [P.sb([128, 512], BF16) for _ in range(2)]
        self.bsq = [Buf() for _ in range(2)]
        self.rt = [P.sb([128, 512], F32) for _ in range(2)]
        self.brt = [Buf() for _ in range(2)]
        self.rs = [P.sb([128, 512], F32) for _ in range(2)]
        self.brs = [Buf() for _ in range(2)]
        self.nrm_i = 0


def load_weight_bf16(P, w_dram, ncols, nchunk=16, stage_cols=None):
    wbf = P.sb([128, nchunk, ncols], BF16)
    bw = Buf()
    st = [P.sb([128, ncols], F32) for _ in range(2)]
    bst = [Buf() for _ in range(2)]
    sst = [P.sem() for _ in range(2)]
    wv = w_dram.rearrange("(c p) n -> p c n", p=128)
    for c in range(nchunk):
        sl = c % 2
        P.dma("sp", st[sl][:], wv[:, c, :], (), [bst[sl]], sst[sl])
        P.copy(("dve", "pool")[c % 2], wbf[:, c, :], st[sl][:], [bst[sl]], [bw])
    return wbf, bw


def rmsnorm_fm(A, pin, bpin, M, nfeat, gain, bgain, out, bout, ssbank, N=512, extra_out=None):
    P = A.P
    i = A.nrm_i % 2
    A.nrm_i += 1
    sq, bsq = A.sq[i], A.bsq[i]
    rt, brt = A.rt[i], A.brt[i]
    rs, brs = A.rs[i], A.brs[i]
    pss, bpss = A.psb[ssbank], A.bps[ssbank]
    P.act(sq[:M, :N], pin, AF.Square, [bpin], [bsq])
    P.mm(pss[:M, :N], A.ones[:M, :M], sq[:M, :N], True, True, [A.bones, bsq], [bpss])
    P.act(rt[:M, :N], pss[:M, :N], AF.Sqrt, [bpss], [brt], bias=float(nfeat * EPS), scale=1.0)
    P.recip(rs[:M, :N], rt[:M, :N], [brt], [brs])
    P.stt("dve", out, pin, gain, rs[:M, :N], ALU.mult, ALU.mult, [bpin, bgain, brs], [bout])
    if extra_out is not None:
        eo, beo = extra_out
        P.stt("dve", eo, pin, gain, rs[:M, :N], ALU.mult, ALU.mult, [bpin, bgain, brs], [beo])


def hT_loader(P, hT, nslots=2):
    hv = hT.rearrange("(c p) t -> p c t", p=128)
    hb = [P.sb([128, 16, 512], BF16) for _ in range(nslots)]
    bhb = [Buf() for _ in range(nslots)]
    sh = [P.sem() for _ in range(nslots)]

    def load(T):
        sl = T % nslots
        for c4 in range(4):
            P.dma("sp", hb[sl][:, 4 * c4:4 * c4 + 4, :], hv[:, 4 * c4:4 * c4 + 4, T * 512:(T + 1) * 512],
                  (), [bhb[sl]], sh[sl])
        return hb[sl], bhb[sl]
    return load


def attn_finalize(A, P, psO, bpsO, psD, bpsD, gate_dram, og_out, Q, wk):
    i = Q % 2
    rd, brd = wk["rd"][i], wk["brd"][i]
    gt, bgt = wk["gt"][i], wk["bgt"][i]
    ot, bot = wk["ot"][i], wk["bot"][i]
    P.dma("sp", gt[:], gate_dram[:, Q * 512:(Q + 1) * 512], (), [bgt], wk["sg"][i])
    P.recip(rd[:], psD[:], [bpsD], [brd])
    P.tt("dve", rd[:], rd[:], gt[:], ALU.mult, [brd, bgt], [brd])
    P.tt("dve", ot[:], psO[:], rd[:], ALU.mult, [bpsO, brd], [bot])
    P.dma("pool", og_out[:, Q * 512:(Q + 1) * 512], ot[:], [bot], (), wk["so"][i], is_output=True)


def attn_work(P):
    wk = {}
    wk["rd"] = [P.sb([128, 512], F32) for _ in range(2)]
    wk["brd"] = [Buf() for _ in range(2)]
    wk["gt"] = [P.sb([128, 512], BF16) for _ in range(2)]
    wk["bgt"] = [Buf() for _ in range(2)]
    wk["ot"] = [P.sb([128, 512], BF16) for _ in range(2)]
    wk["bot"] = [Buf() for _ in range(2)]
    wk["sg"] = [P.sem() for _ in range(2)]
    wk["so"] = [P.sem("pool") for _ in range(2)]
    wk["pt"] = [P.sb([128, 512], BF16) for _ in range(3)]
    wk["bpt"] = [Buf() for _ in range(3)]
    return wk


def dense_attention(A, P, wk, nQ, kts_of, score_fn, V, bV, gate_dram, og_out, exp_bias_fn=None):
    psS = [(A.psb[0], A.bps[0]), (A.psb[1], A.bps[1])]
    psO = [(A.psb[2], A.bps[2]), (A.psb[3], A.bps[3])]
    psD = [(A.psb[4], A.bps[4]), (A.psb[5], A.bps[5])]
    ti = 0
    for Q in range(nQ):
        kts = kts_of(Q)
        po, bpo = psO[Q % 2]
        pd, bpd = psD[Q % 2]
        n = len(kts)
        pend = None
        for j, (kt, c0) in enumerate(kts):
            ps, bps = psS[ti % 2]
            pt, bpt = wk["pt"][ti % 3], wk["bpt"][ti % 3]
            score_fn(ps, bps, Q, kt, c0)
            bias = exp_bias_fn(Q, kt) if exp_bias_fn is not None else None
            P.act(pt[:, c0:512], ps[:, c0:512], AF.Exp, [bps], [bpt], bias=bias)
            if pend is not None:
                pend()
            def pv(kt=kt, c0=c0, pt=pt, bpt=bpt, j=j):
                P.mm(po[:, c0:512], V[:, kt, :], pt[:, c0:512], j == 0, j == n - 1, [bV, bpt], [bpo], sgc=True)
                P.mm(pd[:, c0:512], A.ones[:], pt[:, c0:512], j == 0, j == n - 1, [A.bones, bpt], [bpd], sgc=True)
            pend = pv
            ti += 1
        pend()
        attn_finalize(A, P, po, bpo, pd, bpd, gate_dram, og_out, Q, wk)


def phase_A_mla(P, nc, io):
    A = ACtx(P, nc, io)
    hT = io["hT"]
    Qn = [nc.dram_tensor("scr_qn%d" % h, [128, S], BF16).ap() for h in range(2)]
    Qr = [nc.dram_tensor("scr_qr%d" % h, [64, S], BF16).ap() for h in range(2)]
    Kn = [nc.dram_tensor("scr_kn%d" % h, [128, S], BF16).ap() for h in range(2)]
    Kr = nc.dram_tensor("scr_kr", [64, S], BF16).ap()
    Vd = [nc.dram_tensor("scr_v%d" % h, [S, 128], BF16).ap() for h in range(2)]
    Gd = [nc.dram_tensor("scr_g%d" % h, [128, S], BF16).ap() for h in range(2)]
    bscr = Buf()

    with ExitStack() as es1:
        P.es, es_saved = es1, P.es
        w_in, bw_in = load_weight_bf16(P, io["w_in"], 1088)
        w_qup, bw_qup = load_weight_bf16(P, io["w_qup"], 384, nchunk=4)
        w_kvup, bw_kvup = load_weight_bf16(P, io["w_kvup"], 512, nchunk=2)
        gl = P.sb([128, 12], F32)
        bgl = Buf()
        sgl = P.sem()
        P.dma("sp", gl[:, 0:4], io["g_ql"], (), [bgl], sgl)
        P.dma("sp", gl[:, 4:6], io["g_kvl"], (), [bgl], sgl)
        P.dma("sp", gl[:, 6:8], io["g_q"], (), [bgl], sgl)
        P.dma("sp", gl[:, 8:10], io["g_k"], (), [bgl], sgl)
        gp = P.sb([128, 12], F32)
        bgp = Buf()
        sc = 192.0 ** -0.5
        P.ts("dve", gp[:, 0:4], gl[:, 0:4], math.sqrt(512.0), None, ALU.mult, None, [bgl], [bgp])
        P.ts("dve", gp[:, 4:6], gl[:, 4:6], math.sqrt(256.0), None, ALU.mult, None, [bgl], [bgp])
        P.ts("dve", gp[:, 6:7], gl[:, 6:7], math.sqrt(128.0) * sc, None, ALU.mult, None, [bgl], [bgp])
        P.ts("dve", gp[:, 7:8], gl[:, 7:8], 8.0 * sc, None, ALU.mult, None, [bgl], [bgp])
        P.ts("dve", gp[:, 8:9], gl[:, 8:9], math.sqrt(128.0), None, ALU.mult, None, [bgl], [bgp])
        P.ts("dve", gp[:, 9:10], gl[:, 9:10], 8.0, None, ALU.mult, None, [bgl], [bgp])
        rot = P.sb([64, 64], BF16)
        brot = Buf()
        P.dma("sp", rot[:], io["rot"], (), [brot], P.sem())
        cs = [P.sb([64, 512], F32) for _ in range(2)]
        sn = [P.sb([64, 512], F32) for _ in range(2)]
        bcs = [Buf() for _ in range(2)]
        scs = [P.sem() for _ in range(2)]
        load_h = hT_loader(P, hT)
        ql = P.sb([128, 4, 512], F32)
        bql = Buf()
        qln = P.sb([128, 4, 512], BF16)
        bqln = Buf()
        kvl = P.sb([128, 2, 512], F32)
        bkvl = Buf()
        kvn = P.sb([128, 2, 512], BF16)
        bkvn = Buf()
        sqc = P.sb([128, 4, 512], BF16)
        bsqc = Buf()
        rtl = P.sb([128, 512], F32)
        rsl = P.sb([128, 512], F32)
        brtl, brsl = Buf(), Buf()
        ost = [P.sb([128, 512], BF16) for _ in range(4)]
        bost = [Buf() for _ in range(4)]
        sost = [P.sem("pool") for _ in range(4)]
        oi = [0]
        xr = P.sb([64, 512], F32)
        xrb = P.sb([64, 512], BF16)
        bxr, bxrb = Buf(), Buf()
        t1 = P.sb([64, 512], F32)
        bt1 = Buf()

        def stage_out():
            i = oi[0] % 4
            oi[0] += 1
            return ost[i], bost[i], sost[i]

        def rope_to(pin, bpin, gain_col, dst_dram, T, sl):
            rmsnorm_fm(A, pin, bpin, 64, 64, gp[:64, gain_col:gain_col + 1], bgp, xr[:], bxr, 7,
                       extra_out=(xrb[:], bxrb))
            pr, bpr = A.psb[6], A.bps[6]
            P.mm(pr[:64, :], rot[:], xrb[:], True, True, [brot, bxrb], [bpr])
            P.tt("dve", t1[:], xr[:], cs[sl][:], ALU.mult, [bxr, bcs[sl]], [bt1])
            P.tt("dve", xr[:], pr[:64, :], sn[sl][:], ALU.mult, [bpr, bcs[sl]], [bxr])
            o, bo, so = stage_out()
            P.tt("dve", o[:64, :], t1[:], xr[:], ALU.add, [bt1, bxr], [bo])
            P.dma("pool", dst_dram[:, T * 512:(T + 1) * 512], o[:64, :], [bo], [bscr], so)

        for T in range(S // 512):
            hb, bhb = load_h(T)
            sl = T % 2
            P.dma("sp", cs[sl][:], io["cos"][:, T * 512:(T + 1) * 512], (), [bcs[sl]], scs[sl])
            P.dma("sp", sn[sl][:], io["sin"][:, T * 512:(T + 1) * 512], (), [bcs[sl]], scs[sl])
            for c in range(4):
                pb, bpb = A.psb[c % 2], A.bps[c % 2]
                for k in range(16):
                    P.mm(pb[:], w_in[:, k, c * 128:(c + 1) * 128], hb[:, k, :], k == 0, k == 15, [bw_in, bhb], [bpb])
                P.copy("act", ql[:, c, :], pb[:], [bpb], [bql])
                P.act(sqc[:, c, :], pb[:], AF.Square, [bpb], [bsqc])
            pss, bpss = A.psb[2], A.bps[2]
            for c in range(4):
                P.mm(pss[:], A.ones[:], sqc[:, c, :], c == 0, c == 3, [A.bones, bsqc], [bpss])
            P.act(rtl[:], pss[:], AF.Sqrt, [bpss], [brtl], bias=float(512 * EPS), scale=1.0)
            P.recip(rsl[:], rtl[:], [brtl], [brsl])
            for c in range(4):
                P.stt("dve", qln[:, c, :], ql[:, c, :], gp[:, c:c + 1], rsl[:], ALU.mult, ALU.mult,
                      [bql, bgp, brsl], [bqln])
            for c in range(2):
                pb, bpb = A.psb[c % 2], A.bps[c % 2]
                for k in range(16):
                    P.mm(pb[:], w_in[:, k, 512 + c * 128:512 + (c + 1) * 128], hb[:, k, :], k == 0, k == 15,
                         [bw_in, bhb], [bpb])
                P.copy("act", kvl[:, c, :], pb[:], [bpb], [bkvl])
                P.act(sqc[:, c, :], pb[:], AF.Square, [bpb], [bsqc])
            for c in range(2):
                P.mm(pss[:], A.ones[:], sqc[:, c, :], c == 0, c == 1, [A.bones, bsqc], [bpss])
            P.act(rtl[:], pss[:], AF.Sqrt, [bpss], [brtl], bias=float(256 * EPS), scale=1.0)
            P.recip(rsl[:], rtl[:], [brtl], [brsl])
            for c in range(2):
                P.stt("dve", kvn[:, c, :], kvl[:, c, :], gp[:, 4 + c:5 + c], rsl[:], ALU.mult, ALU.mult,
                      [bkvl, bgp, brsl], [bkvn])
            pb, bpb = A.psb[3], A.bps[3]
            for k in range(16):
                P.mm(pb[:64, :], w_in[:, k, 768:832], hb[:, k, :], k == 0, k == 15, [bw_in, bhb], [bpb])
            rope_to(pb[:64, :], bpb, 9, Kr, T, sl)
            for h in range(2):
                pb, bpb = A.psb[4], A.bps[4]
                for c in range(4):
                    P.mm(pb[:], w_qup[:, c, h * 192:h * 192 + 128], qln[:, c, :], c == 0, c == 3, [bw_qup, bqln], [bpb])
                o, bo, so = stage_out()
                rmsnorm_fm(A, pb[:], bpb, 128, 128, gp[:, 6:7], bgp, o[:], bo, 7)
                P.dma("pool", Qn[h][:, T * 512:(T + 1) * 512], o[:], [bo], [bscr], so)
                pb, bpb = A.psb[5], A.bps[5]
                for c in range(4):
                    P.mm(pb[:64, :], w_qup[:, c, h * 192 + 128:h * 192 + 192], qln[:, c, :], c == 0, c == 3,
                         [bw_qup, bqln], [bpb])
                rope_to(pb[:64, :], bpb, 7, Qr[h], T, sl)
                pb, bpb = A.psb[4], A.bps[4]
                for c in range(2):
                    P.mm(pb[:], w_kvup[:, c, h * 256:h * 256 + 128], kvn[:, c, :], c == 0, c == 1, [bw_kvup, bkvn], [bpb])
                o, bo, so = stage_out()
                rmsnorm_fm(A, pb[:], bpb, 128, 128, gp[:, 8:9], bgp, o[:], bo, 7)
                P.dma("pool", Kn[h][:, T * 512:(T + 1) * 512], o[:], [bo], [bscr], so)
                pb, bpb = A.psb[5], A.bps[5]
                for st in range(4):
                    for c in range(2):
                        P.mm(pb[:, st * 128:(st + 1) * 128], kvn[:, c, st * 128:(st + 1) * 128],
                             w_kvup[:, c, h * 256 + 128:h * 256 + 256], c == 0, c == 1, [bw_kvup, bkvn], [bpb])
                o, bo, so = stage_out()
                P.copy("act", o[:], pb[:], [bpb], [bo])
                P.dma("pool", Vd[h][T * 512:(T + 1) * 512, :].rearrange("(s p) d -> p s d", p=128),
                      o[:].rearrange("p (s d) -> p s d", d=128), [bo], [bscr], so)
                pb, bpb = A.psb[4], A.bps[4]
                for k in range(16):
                    P.mm(pb[:], w_in[:, k, 832 + h * 128:832 + (h + 1) * 128], hb[:, k, :], k == 0, k == 15,
                         [bw_in, bhb], [bpb])
                o, bo, so = stage_out()
                P.act(o[:], pb[:], AF.Silu, [bpb], [bo])
                P.dma("pool", Gd[h][:, T * 512:(T + 1) * 512], o[:], [bo], [bscr], so)
        P.es = es_saved
    P.barrier()

    with ExitStack() as es2:
        P.es, es_saved = es2, P.es
        wk = attn_work(P)
        for h in range(2):
            qn = P.sb([128, S], BF16)
            qr = P.sb([64, S], BF16)
            kn = P.sb([128, S], BF16)
            kr = P.sb([64, S], BF16)
            v = P.sb([128, 64, 128], BF16)
            bq, bk, bv = Buf(), Buf(), Buf()
            sq_, sk_, sv_ = P.sem(), P.sem(), P.sem()
            for q4 in range(4):
                cs_ = slice(q4 * 2048, (q4 + 1) * 2048)
                P.dma("sp", qn[:, cs_], Qn[h][:, cs_], (), [bq], sq_)
                P.dma("sp", kn[:, cs_], Kn[h][:, cs_], (), [bk], sk_)
                P.dma("sp", qr[:, cs_], Qr[h][:, cs_], (), [bq], sq_)
                P.dma("sp", kr[:, cs_], Kr[:, cs_], (), [bk], sk_)
                P.dma("sp", v[:, q4 * 16:(q4 + 1) * 16, :],
                      Vd[h][q4 * 2048:(q4 + 1) * 2048, :].rearrange("(s p) d -> p s d", p=128), (), [bv], sv_)

            def kts_of(Q):
                return [(kt, 0) for kt in range(4 * Q)] + [(4 * Q + i, 128 * i) for i in range(4)]

            def score(ps, bps, Q, kt, c0):
                q0 = Q * 512 + c0
                diag = kt >= 4 * Q
                P.mm(ps[:, c0:512], kn[:, kt * 128:(kt + 1) * 128], qn[:, q0:(Q + 1) * 512], True, False,
                     [bk, bq], [bps])
                P.mm(ps[:, c0:512], kr[:, kt * 128:(kt + 1) * 128], qr[:, q0:(Q + 1) * 512], False, not diag,
                     [bk, bq], [bps])
                if diag:
                    P.mm(ps[:, c0:c0 + 128], A.ident[:], A.negtri[:], False, True, [A.bconst], [bps])

            dense_attention(A, P, wk, S // 512, kts_of, score, v, bv, Gd[h], io["ogT"][h * 128:(h + 1) * 128, :])
        P.es = es_saved


def const_tables():
    c = {}
    c["ident"] = np.eye(128, dtype=np.float32).astype(NPBF)
    s_ = np.arange(128)[:, None]
    t_ = np.arange(128)[None, :]
    c["negtri"] = np.where(s_ > t_, NEG, 0.0).astype(np.float32).astype(NPBF)
    c["negtri_strict"] = np.where(s_ >= t_, NEG, 0.0).astype(np.float32).astype(NPBF)
    inv_freq = (10000.0 ** (-np.arange(0, 64, 2, dtype=np.float32) / np.float32(64))).astype(np.float32)
    ang = (np.arange(S, dtype=np.float32)[:, None] * inv_freq[None, :]).astype(np.float32)
    cos = np.cos(ang).astype(np.float32).T
    sin = np.sin(ang).astype(np.float32).T
    c["cos"] = np.ascontiguousarray(np.concatenate([cos, cos], axis=0))
    c["sin"] = np.ascontiguousarray(np.concatenate([sin, sin], axis=0))
    rot = np.zeros((64, 64), np.float32)
    for m in range(32):
        rot[m + 32, m] = -1.0
        rot[m, m + 32] = 1.0
    c["rot"] = rot.astype(NPBF)
    return c


def col(v, n):
    return np.ascontiguousarray(np.asarray(v, np.float32).reshape(n, 128).T)


def build_A_mla():
    nc = new_nc()
    io = {}

    def inp(name, shape, dt):
        io[name] = nc.dram_tensor(name, list(shape), dt, kind="ExternalInput").ap()
    inp("hT", [D, S], BF16)
    inp("w_in", [D, 1088], F32)
    inp("w_qup", [512, 384], F32)
    inp("w_kvup", [256, 512], F32)
    inp("g_ql", [128, 4], F32)
    inp("g_kvl", [128, 2], F32)
    inp("g_q", [128, 2], F32)
    inp("g_k", [128, 2], F32)
    inp("cos", [64, S], F32)
    inp("sin", [64, S], F32)
    inp("rot", [64, 64], BF16)
    inp("ident", [128, 128], BF16)
    inp("negtri", [128, 128], BF16)
    io["ogT"] = nc.dram_tensor("ogT", [256, S], BF16, kind="ExternalOutput").ap()
    with ExitStack() as es:
        P = Prog(nc, es)
        phase_A_mla(P, nc, io)
        P.emit()
    return nc


def maps_A_mla(inputs, hT, C, cores):
    maps = []
    w_in = inputs["w_in_a"][0]
    for c in cores:
        m = {"hT": hT}
        m["w_in"] = np.ascontiguousarray(np.concatenate([w_in[:, :832], w_in[:, 832 + 256 * c:832 + 256 * (c + 1)]], axis=1))
        m["w_qup"] = np.ascontiguousarray(inputs["w_q_up_a"][0][:, 384 * c:384 * (c + 1)])
        m["w_kvup"] = np.ascontiguousarray(inputs["w_kv_up_a"][0][:, 512 * c:512 * (c + 1)])
        m["g_ql"] = col(inputs["q_lat_norm_a"][0], 4)
        m["g_kvl"] = col(inputs["kv_lat_norm_a"][0], 2)
        gq = np.zeros((128, 2), np.float32)
        gq[:, 0] = inputs["q_norm_a"][0][:128]
        gq[:64, 1] = inputs["q_norm_a"][0][128:]
        gk = np.zeros((128, 2), np.float32)
        gk[:, 0] = inputs["k_norm_a"][0][:128]
        gk[:64, 1] = inputs["k_norm_a"][0][128:]
        m["g_q"], m["g_k"] = gq, gk
        for k in ("cos", "sin", "rot", "ident", "negtri"):
            m[k] = C[k]
        maps.append(m)
    return maps


def stage1_qkvg(A, P, nc, io, scr, bscr, qk_norm, q_scale):
    with ExitStack() as es1:
        P.es, es_saved = es1, P.es
        w_in, bw_in = load_weight_bf16(P, io["w_in"], 1024)
        if qk_norm:
            gl = P.sb([128, 2], F32)
            gp = P.sb([128, 2], F32)
            bgl, bgp = Buf(), Buf()
            sgl = P.sem()
            P.dma("sp", gl[:, 0:1], io["g_q"], (), [bgl], sgl)
            P.dma("sp", gl[:, 1:2], io["g_k"], (), [bgl], sgl)
            P.ts("dve", gp[:, 0:1], gl[:, 0:1], math.sqrt(128.0) * q_scale, None, ALU.mult, None, [bgl], [bgp])
            P.ts("dve", gp[:, 1:2], gl[:, 1:2], math.sqrt(128.0), None, ALU.mult, None, [bgl], [bgp])
        load_h = hT_loader(P, io["hT"])
        ost = [P.sb([128, 512], BF16) for _ in range(4)]
        bost = [Buf() for _ in range(4)]
        sost = [P.sem("pool") for _ in range(4)]
        oi = [0]

        def stage_out():
            i = oi[0] % 4
            oi[0] += 1
            return ost[i], bost[i], sost[i]

        bi = [0]

        def bank():
            i = bi[0] % 4
            bi[0] += 1
            return A.psb[i], A.bps[i]

        for T in range(S // 512):
            hb, bhb = load_h(T)
            ts_ = slice(T * 512, (T + 1) * 512)
            for h in range(2):
                for which, name in ((0, "q"), (1, "k")):
                    pb, bpb = bank()
                    c0 = which * 256 + h * 128
                    for k in range(16):
                        P.mm(pb[:], w_in[:, k, c0:c0 + 128], hb[:, k, :], k == 0, k == 15, [bw_in, bhb], [bpb])
                    o, bo, so = stage_out()
                    if qk_norm:
                        rmsnorm_fm(A, pb[:], bpb, 128, 128, gp[:, which:which + 1], bgp, o[:], bo, A.ssbank)
                    elif which == 0:
                        P.act(o[:], pb[:], AF.Copy, [bpb], [bo], scale=q_scale)
                    else:
                        P.copy("dve", o[:], pb[:], [bpb], [bo])
                    P.dma("pool", scr[name][h][:, ts_], o[:], [bo], [bscr[name][h]], so)
                pb, bpb = bank()
                for st in range(4):
                    for k in range(16):
                        P.mm(pb[:, st * 128:(st + 1) * 128], hb[:, k, st * 128:(st + 1) * 128],
                             w_in[:, k, 512 + h * 128:512 + (h + 1) * 128], k == 0, k == 15, [bw_in, bhb], [bpb])
                o, bo, so = stage_out()
                P.copy("dve", o[:], pb[:], [bpb], [bo])
                P.dma("pool", scr["v"][h][ts_, :].rearrange("(s p) d -> p s d", p=128),
                      o[:].rearrange("p (s d) -> p s d", d=128), [bo], [bscr["v"][h]], so)
                pb, bpb = bank()
                for k in range(16):
                    P.mm(pb[:], w_in[:, k, 768 + h * 128:768 + (h + 1) * 128], hb[:, k, :], k == 0, k == 15,
                         [bw_in, bhb], [bpb])
                o, bo, so = stage_out()
                P.act(o[:], pb[:], AF.Silu, [bpb], [bo])
                P.dma("pool", scr["g"][h][:, ts_], o[:], [bo], [bscr["g"][h]], so)
        P.es = es_saved
    P.barrier()


def make_scratch(nc, tag):
    scr = {"q": [], "k": [], "v": [], "g": []}
    bscr = {"q": [], "k": [], "v": [], "g": []}
    for h in range(2):
        scr["q"].append(nc.dram_tensor("scr%s_q%d" % (tag, h), [128, S], BF16).ap())
        scr["k"].append(nc.dram_tensor("scr%s_k%d" % (tag, h), [128, S], BF16).ap())
        scr["v"].append(nc.dram_tensor("scr%s_v%d" % (tag, h), [S, 128], BF16).ap())
        scr["g"].append(nc.dram_tensor("scr%s_g%d" % (tag, h), [128, S], BF16).ap())
        for k in bscr:
            bscr[k].append(Buf())
    return scr, bscr


def load_qkv(P, scr, h, bq, bk, bv):
    qn = P.sb([128, S], BF16)
    kn = P.sb([128, S], BF16)
    v = P.sb([128, 64, 128], BF16)
    sq_, sk_, sv_ = P.sem(), P.sem(), P.sem()
    for q4 in range(4):
        cs_ = slice(q4 * 2048, (q4 + 1) * 2048)
        P.dma("sp", qn[:, cs_], scr["q"][h][:, cs_], (), [bq], sq_)
        P.dma("sp", kn[:, cs_], scr["k"][h][:, cs_], (), [bk], sk_)
        P.dma("sp", v[:, q4 * 16:(q4 + 1) * 16, :],
              scr["v"][h][q4 * 2048:(q4 + 1) * 2048, :].rearrange("(s p) d -> p s d", p=128), (), [bv], sv_)
    return qn, kn, v


def phase_A_sb(P, nc, io, tag="sb"):
    A = ACtx(P, nc, io)
    scr, bscr = make_scratch(nc, tag)
    stage1_qkvg(A, P, nc, io, scr, bscr, qk_norm=False, q_scale=128.0 ** -0.5)
    with ExitStack() as es2:
        P.es, es_saved = es2, P.es
        wk = attn_work(P)
        negU = P.sb([128, 128], BF16)
        negones = P.sb([128, 128], BF16)
        mstrict = P.sb([128, 128], BF16)
        bc2 = Buf()
        sc2 = P.sem()
        P.dma("sp", negU[:], io["negU"], (), [bc2], sc2)
        P.dma("sp", mstrict[:], io["mstrict"], (), [bc2], sc2)
        P.memset("pool", negones[:], -1.0, [bc2])
        negtri_s = P.sb([128, 128], BF16)
        P.dma("sp", negtri_s[:], io["negtri_strict"], (), [bc2], sc2)
        et = [P.sb([128, 512], F32) for _ in range(2)]
        bet = [Buf() for _ in range(2)]
        lt = [P.sb([128, 512], BF16) for _ in range(3)]
        blt = [Buf() for _ in range(3)]
        R32 = P.sb([128, 512], F32)
        Rb = P.sb([128, 512], BF16)
        bR32, bRb = Buf(), Buf()
        psZ = [(A.psb[i], A.bps[i]) for i in (0, 1, 2)]
        psB = [(A.psb[i], A.bps[i]) for i in (3, 4)]
        psO = [(A.psb[i], A.bps[i]) for i in (5, 6)]
        zi = [0]
        for h in range(2):
            bq, bk, bv = Buf(), Buf(), Buf()
            qn, kn, v = load_qkv(P, scr, h, bq, bk, bv)
            for Q in range(S // 512):
                tiles = [(4 * Q + i, 128 * i) for i in (3, 2, 1, 0)] + [(kt, 0) for kt in range(4 * Q - 1, -1, -1)]
                n = len(tiles)
                po, bpo = psO[Q % 2]
                P.memset("pool", R32[:], 0.0, [bR32])
                P.memset("pool", Rb[:], 0.0, [bRb])
                st = {}

                def Z(t):
                    kt, c0 = tiles[t]
                    pz, bpz = psZ[(zi[0] + t) % 3]
                    P.mm(pz[:, c0:512], kn[:, kt * 128:(kt + 1) * 128], qn[:, Q * 512 + c0:(Q + 1) * 512], True, True,
                         [bk, bq], [bpz])

                def EL(t):
                    kt, c0 = tiles[t]
                    pz, bpz = psZ[(zi[0] + t) % 3]
                    e, be = et[t % 2], bet[t % 2]
                    l, bl = lt[t % 3], blt[t % 3]
                    P.act(e[:, c0:512], pz[:, c0:512], AF.Exp, [bpz], [be])
                    P.act(l[:, c0:512], e[:, c0:512], AF.Ln, [be], [bl], bias=1.0)
                    if kt >= 4 * Q:
                        P.tt("dve", l[:, c0:c0 + 128], l[:, c0:c0 + 128], mstrict[:], ALU.mult, [bl, bc2], [bl])

                def Bm(t):
                    kt, c0 = tiles[t]
                    l, bl = lt[t % 3], blt[t % 3]
                    pb, bpb = psB[t % 2]
                    diag = kt >= 4 * Q
                    P.mm(pb[:, c0:512], kn[:, kt * 128:(kt + 1) * 128], qn[:, Q * 512 + c0:(Q + 1) * 512], True, False,
                         [bk, bq], [bpb])
                    last = (t == 0) and not diag
                    P.mm(pb[:, c0:512], negU[:], l[:, c0:512], False, last, [bc2, bl], [bpb])
                    if t > 0:
                        P.mm(pb[:, c0:512], negones[:], Rb[:, c0:512], False, not diag, [bc2, bRb], [bpb])
                    if diag:
                        P.mm(pb[:, c0:c0 + 128], A.ident[:], negtri_s[:], False, True, [A.bconst, bc2], [bpb])

                def Rupd(t):
                    kt, c0 = tiles[t]
                    if t == n - 1:
                        return
                    l, bl = lt[t % 3], blt[t % 3]
                    P.tt("dve", R32[:, c0:512], R32[:, c0:512], l[:, c0:512], ALU.add, [bR32, bl], [bR32])
                    P.copy("pool", Rb[:, c0:512], R32[:, c0:512], [bR32], [bRb])

                def Aexp(t):
                    kt, c0 = tiles[t]
                    pb, bpb = psB[t % 2]
                    pt, bpt = wk["pt"][t % 3], wk["bpt"][t % 3]
                    P.act(pt[:, c0:512], pb[:, c0:512], AF.Exp, [bpb], [bpt])

                def PV(t):
                    kt, c0 = tiles[t]
                    pt, bpt = wk["pt"][t % 3], wk["bpt"][t % 3]
                    P.mm(po[:, c0:512], v[:, kt, :], pt[:, c0:512], t == 0, t == n - 1, [bv, bpt], [bpo], sgc=True)

                Z(0)
                for t in range(n):
                    if t + 1 < n:
                        Z(t + 1)
                    EL(t)
                    if t >= 1:
                        Aexp(t - 1)
                    Bm(t)
                    Rupd(t)
                    if t >= 1:
                        PV(t - 1)
                Aexp(n - 1)
                PV(n - 1)
                zi[0] += n
                i = Q % 2
                gt, bgt = wk["gt"][i], wk["bgt"][i]
                ot, bot = wk["ot"][i], wk["bot"][i]
                P.dma("sp", gt[:], scr["g"][h][:, Q * 512:(Q + 1) * 512], (), [bgt], wk["sg"][i])
                P.tt("dve", ot[:], po[:], gt[:], ALU.mult, [bpo, bgt], [bot])
                P.dma("pool", io["ogT"][h * 128:(h + 1) * 128, Q * 512:(Q + 1) * 512], ot[:], [bot], (), wk["so"][i],
                      is_output=True)
        P.es = es_saved


def std_inputs(nc, io, names):
    for name, shape, dt in names:
        io[name] = nc.dram_tensor(name, list(shape), dt, kind="ExternalInput").ap()


def build_A_sb():
    nc = new_nc()
    io = {}
    std_inputs(nc, io, [("hT", [D, S], BF16), ("w_in", [D, 1024], F32), ("ident", [128, 128], BF16),
                        ("negtri", [128, 128], BF16), ("negtri_strict", [128, 128], BF16),
                        ("negU", [128, 128], BF16), ("mstrict", [128, 128], BF16)])
    io["ogT"] = nc.dram_tensor("ogT", [256, S], BF16, kind="ExternalOutput").ap()
    with ExitStack() as es:
        P = Prog(nc, es)
        phase_A_sb(P, nc, io)
        P.emit()
    return nc


def w4_slice(w, c):
    return np.ascontiguousarray(np.concatenate([w[:, q * 2048 + 256 * c:q * 2048 + 256 * (c + 1)] for q in range(4)], axis=1))


def sb_consts(C):
    if "negU" not in C:
        j = np.arange(128)[:, None]
        s_ = np.arange(128)[None, :]
        C["negU"] = np.where(j >= s_, -1.0, 0.0).astype(np.float32).astype(NPBF)
        C["mstrict"] = np.where(j < s_, 1.0, 0.0).astype(np.float32).astype(NPBF)
    return C


def maps_A_sb(inputs, hT, C, cores):
    sb_consts(C)
    maps = []
    for c in cores:
        m = {"hT": hT, "w_in": w4_slice(inputs["w_in_c"][0], c)}
        for k in ("ident", "negtri", "negtri_strict", "negU", "mstrict"):
            m[k] = C[k]
        maps.append(m)
    return maps


def bf16_split3(x):
    hi = np.float32(x).astype(NPBF)
    r1 = np.float32(x) - hi.astype(np.float32)
    mid = r1.astype(NPBF)
    r2 = r1 - mid.astype(np.float32)
    lo = r2.astype(NPBF)
    return float(hi.astype(np.float32)), float(mid.astype(np.float32)), float(lo.astype(np.float32))


def alibi_slope(hd):
    return 2.0 ** (-8.0 * (hd + 1) / 16.0)


def alibi_rows(hd, key_tile=128):
    hi, mid, lo = bf16_split3(alibi_slope(hd))
    lhs = np.zeros((15, 64, 128), np.float32)
    rhs = np.zeros((15, S), np.float32)
    m = np.arange(128, dtype=np.float32)
    t = np.arange(S)
    for j, p in enumerate((hi, mid, lo)):
        lhs[0 + j, :, :] = m[None, :]
        rhs[0 + j, :] = p
        lhs[3 + j, :, :] = np.arange(64, dtype=np.float32)[:, None]
        rhs[3 + j, :] = 128.0 * p
        lhs[6 + j, :, :] = p
        rhs[6 + j, :] = -(512.0 * (t // 512))
        lhs[9 + j, :, :] = p
        rhs[9 + j, :] = -(256.0 * ((t % 512) // 256))
        lhs[12 + j, :, :] = p
        rhs[12 + j, :] = -(t % 256).astype(np.float32)
    return lhs, rhs


def moba_tables(hd):
    lhs = np.zeros((47, 64, 128), np.float32)
    for kt in range(64):
        lhs[kt // 2, kt, :] = 1.0
    al, ar = alibi_rows(hd)
    lhs[32:47] = al
    return lhs.astype(NPBF), ar.astype(NPBF)


def phase_A_moba(P, nc, io, tag="mb"):
    A = ACtx(P, nc, io, bf16_bank=True)
    scr, bscr = make_scratch(nc, tag)
    stage1_qkvg(A, P, nc, io, scr, bscr, qk_norm=True, q_scale=128.0 ** -0.5)
    with ExitStack() as es2:
        P.es, es_saved = es2, P.es
        wk = attn_work(P)
        NB = 4
        scs = [P.sb([128, 32], F32, name='scs%d' % i) for i in range(NB)]
        mx = [P.sb([128, 8], F32, name='mx%d' % i) for i in range(NB)]
        negm = [P.sb([128, 32], BF16) for _ in range(NB)]
        bscs = [Buf() for _ in range(NB)]
        bmx = [Buf() for _ in range(NB)]
        bnegm = [Buf() for _ in range(NB)]
        km32 = P.sb([128, 32], F32)
        kmb = P.sb([128, 32], BF16)
        bkm32, bkmb = Buf(), Buf()
        pssc, bpssc = A.psb[6], [Buf() for _ in range(NB)]
        for h in range(2):
            bq, bk, bv = Buf(), Buf(), Buf()
            qn, kn, v = load_qkv(P, scr, h, bq, bk, bv)
            for i in range(NB):
                P.memset("pool", scs[i][:], -1e30, [bscs[i]])
                P.memset("pool", negm[i][:], NEG, [bnegm[i]])
            lhsE = P.sb([47, 64, 128], BF16)
            rhsb = P.sb([47, S], BF16, name='rhsb_h%d' % h)
            blhsE, brhsb = Buf(), Buf()
            st_ = P.sem()
            P.dma("sp", lhsE[:], io["lhsE"][h], (), [blhsE], st_)
            P.dma("sp", rhsb[32:47, :], io["rhsc"][h], (), [brhsb], P.sem())
            P.op("dve", lambda e, kn=kn: e.tensor_reduce(km32[:], kn[:].rearrange("p (n k) -> p n k", k=256),
                                                         mybir.AxisListType.X, ALU.add), [bk], [bkm32])
            P.ts("dve", kmb[:], km32[:], 1.0 / 256.0, None, ALU.mult, None, [bkm32], [bkmb])
            bpssc1, bpst1 = bpssc[0], A.bpst[0]
            for Qg in range(16):
                for i in range(4):
                    qt = Qg * 4 + i
                    blk = qt // 2
                    if blk >= 1:
                        P.mm(pssc[:, i * 32:(i + 1) * 32], qn[:, qt * 128:(qt + 1) * 128], kmb[:], True, True,
                             [bq, bkmb], [bpssc1])
                for i in range(4):
                    qt = Qg * 4 + i
                    blk = qt // 2
                    if blk >= 1:
                        P.copy("dve", scs[i][:, 0:blk], pssc[:, i * 32:i * 32 + blk], [bpssc1], [bscs[i]])
                        P.op("dve", lambda e, i=i: e.max(mx[i][:], scs[i][:]), [bscs[i]], [bmx[i]])
                        P.ts("dve", negm[i][:, 0:blk], scs[i][:, 0:blk], mx[i][:, 2:3], NEG, ALU.is_lt, ALU.mult,
                             [bscs[i], bmx[i]], [bnegm[i]])
                    P.memset("dve", negm[i][:, blk:blk + 1], 0.0, [bnegm[i]])
                for i in range(4):
                    P.tr(A.pst[:32, i * 128:(i + 1) * 128], negm[i][:], A.ident[:], [bnegm[i], A.bconst], [bpst1])
                P.copy("act", rhsb[0:32, Qg * 512:(Qg + 1) * 512], A.pst[:32, 0:512], [bpst1], [brhsb])

            def kts_of(Q):
                return [(kt, 0) for kt in range(4 * Q)] + [(4 * Q + i, 128 * i) for i in range(4)]

            def score(ps, bps, Q, kt, c0, kn=kn, qn=qn, bk=bk, bq=bq, lhsE=lhsE, rhsb=rhsb, blhsE=blhsE, brhsb=brhsb):
                q0 = Q * 512 + c0
                diag = kt >= 4 * Q
                P.mm(ps[:, c0:512], kn[:, kt * 128:(kt + 1) * 128], qn[:, q0:(Q + 1) * 512], True, False,
                     [bk, bq], [bps])
                P.mm(ps[:, c0:512], lhsE[:, kt, :], rhsb[:, q0:(Q + 1) * 512], False, not diag,
                     [blhsE, brhsb], [bps])
                if diag:
                    P.mm(ps[:, c0:c0 + 128], A.ident[:], A.negtri[:], False, True, [A.bconst], [bps])

            dense_attention(A, P, wk, S // 512, kts_of, score, v, bv, scr["g"][h], io["ogT"][h * 128:(h + 1) * 128, :])
        P.es = es_saved


def build_A_moba():
    nc = new_nc()
    io = {}
    std_inputs(nc, io, [("hT", [D, S], BF16), ("w_in", [D, 1024], F32), ("ident", [128, 128], BF16),
                        ("negtri", [128, 128], BF16), ("g_q", [128, 1], F32), ("g_k", [128, 1], F32),
                        ("lhsE", [2, 47, 64, 128], BF16), ("rhsc", [2, 15, S], BF16)])
    io["ogT"] = nc.dram_tensor("ogT", [256, S], BF16, kind="ExternalOutput").ap()
    with ExitStack() as es:
        P = Prog(nc, es)
        phase_A_moba(P, nc, io)
        P.emit()
    return nc


def maps_A_moba(inputs, hT, C, cores):
    maps = []
    for c in cores:
        m = {"hT": hT, "w_in": w4_slice(inputs["w_in_b"][0], c)}
        for k in ("ident", "negtri"):
            m[k] = C[k]
        m["g_q"] = np.ascontiguousarray(inputs["q_norm_b"][0].reshape(128, 1))
        m["g_k"] = np.ascontiguousarray(inputs["k_norm_b"][0].reshape(128, 1))
        tabs = [moba_tables(2 * c + h) for h in range(2)]
        m["lhsE"] = np.ascontiguousarray(np.stack([t[0] for t in tabs]))
        m["rhsc"] = np.ascontiguousarray(np.stack([t[1] for t in tabs]))
        maps.append(m)
    return maps


def nsa_cmp_tables(hd):
    hi, mid, lo = bf16_split3(alibi_slope(hd))
    lhs = np.zeros((18, 4, 128), np.float32)
    rhs = np.zeros((18, S), np.float32)
    m = np.arange(128, dtype=np.float32)
    t = np.arange(S)
    for j, p in enumerate((hi, mid, lo)):
        lhs[0 + j, :, :] = 16.0 * m[None, :]
        rhs[0 + j, :] = p
        lhs[3 + j, :, :] = 2048.0 * np.arange(4, dtype=np.float32)[:, None]
        rhs[3 + j, :] = p
        lhs[6 + j, :, :] = 31.0
        rhs[6 + j, :] = p
        lhs[9 + j, :, :] = p
        rhs[9 + j, :] = -(512.0 * (t // 512))
        lhs[12 + j, :, :] = p
        rhs[12 + j, :] = -(256.0 * ((t % 512) // 256))
        lhs[15 + j, :, :] = p
        rhs[15 + j, :] = -(t % 256).astype(np.float32)
    return lhs.astype(NPBF), rhs.astype(NPBF)


def nsa_const_tables():
    c = {}
    m = np.arange(128)[:, None]
    tl = np.arange(512)[None, :]
    mk = np.zeros((128, 5, 512), np.float32)
    for r in range(5):
        mk[:, r, :] = np.where(tl - 16 * m >= 31 - 512 * r, 0.0, NEG)
    c["maskc"] = mk.astype(NPBF)
    C = np.zeros((128, 4, 129), np.float32)
    for ct in range(4):
        for mm_ in range(128):
            i = 128 * ct + mm_
            if i > 510:
                continue
            for j in range(128):
                if 4 * j - 1 <= i <= 4 * j + 3:
                    C[mm_, ct, j] = 1.0
            C[mm_, ct, 128] = 1.0
    c["cmap"] = C.astype(NPBF)
    G = (np.arange(S)[None, :] // 64 == np.arange(128)[:, None]).astype(np.float32)
    c["gsel"] = G.astype(NPBF)
    s_ = np.arange(128)[:, None]
    u_ = np.arange(128)[None, :]
    c["negtri_lo"] = np.where(u_ < s_, 0.0, NEG).astype(np.float32).astype(NPBF)
    sel = np.zeros((12, 6, 128), np.float32)
    for k in range(12):
        sel[k, k % 6, :] = 1.0
    c["gsel6"] = sel.astype(NPBF)
    return c


def nsa_head_tables(hd):
    hi, mid, lo = bf16_split3(alibi_slope(hd))
    lhs = np.zeros((9, 128), np.float32)
    rhs = np.zeros((9, 512), np.float32)
    m = np.arange(128, dtype=np.float32)
    t = np.arange(512)
    for j, p in enumerate((hi, mid, lo)):
        lhs[0 + j, :] = m
        rhs[0 + j, :] = p
        lhs[3 + j, :] = p
        rhs[3 + j, :] = -(256.0 * (t // 256))
        lhs[6 + j, :] = p
        rhs[6 + j, :] = -(t % 256).astype(np.float32)
    sl = np.float64(hi) + np.float64(mid) + np.float64(lo)
    Qs = np.arange(16)[:, None]
    kts = np.arange(64)[None, :]
    bt = (-sl * (512.0 * Qs - 128.0 * kts)).astype(np.float32).reshape(1, 1024)
    bt = np.maximum(bt, -60000.0)
    bias = np.ascontiguousarray(np.broadcast_to(bt, (128, 1024))).astype(np.float32)
    return lhs.astype(NPBF), rhs.astype(NPBF), bias


def phase_A_nsa(P, nc, io, tag="ns"):
    A = ACtx(P, nc, io, bf16_bank=True)
    own = io["own_p"]
    D_ = {}

    def scr(name, shape, dt=BF16):
        D_[name] = nc.dram_tensor("scr%s_%s" % (tag, name), list(shape), dt).ap()
        return D_[name]
    for p in range(4):
        scr("q%d" % p, [128, S])
    for nm in ("kc", "vc", "ks", "kw"):
        scr(nm, [128, S])
    scr("vs", [S, 128])
    scr("vw", [S, 128])
    for h in range(2):
        scr("g%d" % h, [128, S])
        scr("oc%d" % h, [128, S])
    scr("sgh", [6, S])
    scr("sgl", [6, S])
    scr("negmT", [128, S])
    B_ = {k: Buf() for k in D_}

    with ExitStack() as es1:
        P.es, es_saved = es1, P.es
        w_in, bw_in = load_weight_bf16(P, io["w_in"], 1542)
        gl = P.sb([128, 4], F32)
        gp = P.sb([128, 4], F32)
        bgl, bgp = Buf(), Buf()
        P.dma("sp", gl[:], io["g_qk"], (), [bgl], P.sem())
        P.ts("dve", gp[:, 0:1], gl[:, 0:1], 1.0, None, ALU.mult, None, [bgl], [bgp])
        P.ts("dve", gp[:, 1:4], gl[:, 1:4], math.sqrt(128.0), None, ALU.mult, None, [bgl], [bgp])
        load_h = hT_loader(P, io["hT"])
        ost = [P.sb([128, 512], BF16) for _ in range(4)]
        bost = [Buf() for _ in range(4)]
        sost = [P.sem("pool") for _ in range(4)]
        oi = [0]
        sg32 = P.sb([6, 512], F32)
        bsg32 = Buf()

        def stage_out():
            i = oi[0] % 4
            oi[0] += 1
            return ost[i], bost[i], sost[i]
        bi = [0]

        def bank():
            i = bi[0] % 4
            bi[0] += 1
            return A.psb[i], A.bps[i]

        def proj(col0, M, hb, bhb):
            pb, bpb = bank()
            for k in range(16):
                P.mm(pb[:M, :], w_in[:, k, col0:col0 + M], hb[:, k, :], k == 0, k == 15, [bw_in, bhb], [bpb])
            return pb, bpb

        def proj_tm(col0, hb, bhb):
            pb, bpb = bank()
            for st in range(4):
                for k in range(16):
                    P.mm(pb[:, st * 128:(st + 1) * 128], hb[:, k, st * 128:(st + 1) * 128],
                         w_in[:, k, col0:col0 + 128], k == 0, k == 15, [bw_in, bhb], [bpb])
            return pb, bpb

        for T in range(S // 512):
            hb, bhb = load_h(T)
            ts_ = slice(T * 512, (T + 1) * 512)
            for p in range(4):
                pb, bpb = proj(p * 128, 128, hb, bhb)
                o, bo, so = stage_out()
                rmsnorm_fm(A, pb[:], bpb, 128, 128, gp[:, 0:1], bgp, o[:], bo, A.ssbank)
                P.dma("pool", D_["q%d" % p][:, ts_], o[:], [bo], [B_["q%d" % p]], so)
            for nm, c0 in (("kc", 512), ("vc", 640)):
                pb, bpb = proj(c0, 128, hb, bhb)
                o, bo, so = stage_out()
                P.copy("act", o[:], pb[:], [bpb], [bo])
                P.dma("pool", D_[nm][:, ts_], o[:], [bo], [B_[nm]], so)
            for nm, c0, gi in (("ks", 768, 2), ("kw", 1024, 3)):
                pb, bpb = proj(c0, 128, hb, bhb)
                o, bo, so = stage_out()
                rmsnorm_fm(A, pb[:], bpb, 128, 128, gp[:, gi:gi + 1], bgp, o[:], bo, A.ssbank)
                P.dma("pool", D_[nm][:, ts_], o[:], [bo], [B_[nm]], so)
            for nm, c0 in (("vs", 896), ("vw", 1152)):
                pb, bpb = proj_tm(c0, hb, bhb)
                o, bo, so = stage_out()
                P.copy("dve", o[:], pb[:], [bpb], [bo])
                P.dma("pool", D_[nm][ts_, :].rearrange("(s p) d -> p s d", p=128),
                      o[:].rearrange("p (s d) -> p s d", d=128), [bo], [B_[nm]], so)
            for h in range(2):
                pb, bpb = proj(1280 + h * 128, 128, hb, bhb)
                o, bo, so = stage_out()
                P.act(o[:], pb[:], AF.Silu, [bpb], [bo])
                P.dma("pool", D_["g%d" % h][:, ts_], o[:], [bo], [B_["g%d" % h]], so)
            pb, bpb = proj(1536, 6, hb, bhb)
            P.act(sg32[:], pb[:6, :], AF.Sigmoid, [bpb], [bsg32])
            o, bo, so = stage_out()
            P.copy("dve", o[:6, :], sg32[:], [bsg32], [bo])
            P.dma("pool", D_["sgh"][:, ts_], o[:6, :], [bo], [B_["sgh"]], so)
            o2, bo2, so2 = stage_out()
            P.tt("dve", o2[:6, :], sg32[:], o[:6, :], ALU.subtract, [bsg32, bo], [bo2])
            P.dma("pool", D_["sgl"][:, ts_], o2[:6, :], [bo2], [B_["sgl"]], so2)
        P.es = es_saved
    P.barrier()

    with ExitStack() as es2:
        P.es, es_saved = es2, P.es
        wck, bwck = load_weight_bf16(P, io["w_cmp_k"], 128, nchunk=32)
        wcv, bwcv = load_weight_bf16(P, io["w_cmp_v"], 128, nchunk=32)
        kcT = P.sb([128, S], BF16)
        vcT = P.sb([128, S], BF16)
        bkcT, bvcT = Buf(), Buf()
        s1_, s2_ = P.sem(), P.sem()
        for q4 in range(4):
            cs_ = slice(q4 * 2048, (q4 + 1) * 2048)
            P.dma("sp", kcT[:, cs_], D_["kc"][:, cs_], (), [bkcT], s1_)
            P.dma("sp", vcT[:, cs_], D_["vc"][:, cs_], (), [bvcT], s2_)
        pos32 = P.sb([128, 32], F32)
        posb = P.sb([128, 32], BF16)
        bpos32, bposb = Buf(), Buf()
        P.dma("sp", pos32[:], io["posT"], (), [bpos32], P.sem())
        P.copy("dve", posb[:], pos32[:], [bpos32], [bposb])
        cmap = P.sb([128, 4, 129], BF16)
        maskc = P.sb([128, 5, 512], BF16)
        bcm = Buf()
        scm = P.sem()
        P.dma("sp", cmap[:], io["cmap"], (), [bcm], scm)
        P.dma("sp", maskc[:], io["maskc"], (), [bcm], scm)
        lhsC = P.sb([18, 4, 4, 128], BF16)
        blhsC = Buf()
        P.dma("sp", lhsC[:], io["lhsC"], (), [blhsC], P.sem())
        gkf = P.sb([128, 4], F32)
        gk0 = P.sb([128, 2], F32)
        bgkf, bgk0 = Buf(), Buf()
        P.dma("sp", gkf[:], io["g_qk"], (), [bgkf], P.sem())
        P.ts("dve", gk0[:, 1:2], gkf[:, 1:2], math.sqrt(128.0), None, ALU.mult, None, [bgkf], [bgk0])

        kcmpT = P.sb([128, 512], BF16)
        vcmpT = P.sb([128, 512], BF16)
        vcmp = P.sb([128, 4, 128], BF16)
        bkcmpT, bvcmpT, bvcmp = Buf(), Buf(), Buf()
        P.memset("pool", kcmpT[:], 0.0, [bkcmpT])
        P.memset("pool", vcmpT[:], 0.0, [bvcmpT])
        c32 = P.sb([128, 512], F32)
        bc32 = Buf()
        cst = P.sb([128, 2], F32)
        bcst = Buf()
        kc_v = kcT[:].rearrange("p (i s) -> p i s", s=16)
        vc_v = vcT[:].rearrange("p (i s) -> p i s", s=16)
        for which, (src_v, bsrc, wc, bwc) in enumerate(((kc_v, bkcT, wck, bwck), (vc_v, bvcT, wcv, bwcv))):
            pb, bpb = A.psb[which], A.bps[which]
            for l in range(32):
                a, b = l // 16, l % 16
                P.mm(pb[:, 0:511], wc[:, l, :], src_v[:, a:a + 511, b], l == 0, l == 31, [bwc, bsrc], [bpb])
            pc, bpc = A.psb[2 + which], A.bps[2 + which]
            for l in range(32):
                P.mm(pc[:, 0:1], wc[:, l, :], posb[:, l:l + 1], l == 0, l == 31, [bwc, bposb], [bpc])
            P.copy("dve", cst[:, which:which + 1], pc[:, 0:1], [bpc], [bcst])
            if which == 0:
                P.ts("dve", c32[:, 0:511], pb[:, 0:511], cst[:, 0:1], None, ALU.add, None, [bpb, bcst], [bc32])
                rmsnorm_fm(A, c32[:, 0:511], bc32, 128, 128, gk0[:, 1:2], bgk0, kcmpT[:, 0:511], bkcmpT, A.ssbank, N=511)
            else:
                P.ts("dve", vcmpT[:, 0:511], pb[:, 0:511], cst[:, 1:2], None, ALU.add, None, [bpb, bcst], [bvcmpT])
        for ct in range(4):
            P.tr(A.pst[:, ct * 128:(ct + 1) * 128], vcmpT[:, ct * 128:(ct + 1) * 128], A.ident[:],
                 [bvcmpT, A.bconst], [A.bpst[0]])
        P.copy("act", vcmp[:].rearrange("p c d -> p (c d)"), A.pst[:, 0:512], [A.bpst[0]], [bvcmp])

        wk = attn_work(P)
        qs = [P.sb([128, 512], BF16) for _ in range(2)]
        bqs = [Buf() for _ in range(2)]
        sqs = [P.sem() for _ in range(2)]
        rc = [P.sb([18, 512], BF16) for _ in range(2)]
        brc = [Buf() for _ in range(2)]
        src_ = [P.sem() for _ in range(2)]
        acc = [P.sb([128, 128], F32) for _ in range(4)]
        bacc = [Buf() for _ in range(4)]
        dn = P.sb([128, 4], F32)
        bdn = Buf()
        wsel = [P.sb([128, 128], F32) for _ in range(2)]
        bwsel = [Buf() for _ in range(2)]
        mx1 = P.sb([128, 8], F32)
        mx2 = P.sb([128, 8], F32)
        bmx1, bmx2 = Buf(), Buf()
        w2 = P.sb([128, 128], F32)
        bw2 = Buf()
        ngm = [P.sb([128, 128], BF16) for _ in range(2)]
        bngm = [Buf() for _ in range(2)]
        nst = [P.sb([128, 512], BF16) for _ in range(2)]
        bnst = [Buf() for _ in range(2)]
        snst = [P.sem("pool") for _ in range(2)]
        psS = [(A.psb[0], A.bps[0]), (A.psb[1], A.bps[1])]
        psO, bpsO = A.psb[2], A.bps[2]
        psD, bpsD = A.psb[3], A.bps[3]
        psI = [(A.psb[4], A.bps[4]), (A.psb[5], A.bps[5])]
        BIG = 1.0e30
        ti = 0
        li = 0
        for Q in range(S // 512):
            ncts = Q // 4 + 1
            for p in range(4):
                sl = li % 2
                li += 1
                P.dma("sp", qs[sl][:], D_["q%d" % p][:, Q * 512:(Q + 1) * 512], (), [bqs[sl]], sqs[sl])
                P.dma("sp", rc[sl][:], io["rhsC"][p][:, Q * 512:(Q + 1) * 512], (), [brc[sl]], src_[sl])
                is_own = p in own
                for ct in range(ncts):
                    ps, bps = psS[ti % 2]
                    pt, bpt = wk["pt"][ti % 3], wk["bpt"][ti % 3]
                    ti += 1
                    r = Q - 4 * ct
                    P.mm(ps[:], kcmpT[:, ct * 128:(ct + 1) * 128], qs[sl][:], True, False, [bkcmpT, bqs[sl]], [bps])
                    P.mm(ps[:], lhsC[:, p, ct, :], rc[sl][:], False, r > 4, [blhsC, brc[sl]], [bps])
                    if r <= 4:
                        P.mm(ps[:], A.ident[:], maskc[:, r, :], False, True, [A.bconst, bcm], [bps])
                    P.act(pt[:], ps[:], AF.Exp, [bps], [bpt])
                    if is_own:
                        P.mm(psO[:], vcmp[:, ct, :], pt[:], ct == 0, ct == ncts - 1, [bvcmp, bpt], [bpsO])
                        P.mm(psD[:], A.ones[:], pt[:], ct == 0, ct == ncts - 1, [A.bones, bpt], [bpsD])
                    for sb in range(4):
                        pI, bpI = psI[sb // 2]
                        o_ = (sb % 2) * 129
                        P.mm(pI[:, o_:o_ + 129], pt[:, sb * 128:(sb + 1) * 128], cmap[:, ct, :],
                             ct == 0 and sb % 2 == 0, ct == ncts - 1 and sb % 2 == 1, [bpt, bcm], [bpI])
                for sb in range(4):
                    pI, bpI = psI[sb // 2]
                    o_ = (sb % 2) * 129
                    P.ts("dve", dn[:, sb:sb + 1], pI[:, o_ + 128:o_ + 129], 1e-30, None, ALU.max, None, [bpI], [bdn])
                P.recip(dn[:], dn[:], [bdn], [bdn])
                for sb in range(4):
                    pI, bpI = psI[sb // 2]
                    o_ = (sb % 2) * 129
                    if p == 0:
                        P.ts("dve", acc[sb][:], pI[:, o_:o_ + 128], dn[:, sb:sb + 1], None, ALU.mult, None,
                             [bpI, bdn], [bacc[sb]])
                    else:
                        P.stt("dve", acc[sb][:], pI[:, o_:o_ + 128], dn[:, sb:sb + 1], acc[sb][:], ALU.mult, ALU.add,
                              [bpI, bdn, bacc[sb]], [bacc[sb]])
                if is_own:
                    h = own.index(p)
                    i2 = Q % 2
                    rd, brd = wk["rd"][i2], wk["brd"][i2]
                    ot, bot = wk["ot"][i2], wk["bot"][i2]
                    P.ts("dve", rd[:], psD[:], 1e-30, None, ALU.max, None, [bpsD], [brd])
                    P.recip(rd[:], rd[:], [brd], [brd])
                    P.tt("dve", ot[:], psO[:], rd[:], ALU.mult, [bpsO, brd], [bot])
                    P.dma("pool", D_["oc%d" % h][:, Q * 512:(Q + 1) * 512], ot[:], [bot], [B_["oc%d" % h]], wk["so"][i2])
            nsl = Q % 2
            for sb in range(4):
                tb = 4 * Q + sb
                c_lo, c_hi = 2 * tb, 2 * tb + 1
                w_, bw_ = wsel[sb % 2], bwsel[sb % 2]
                P.copy("pool", w_[:], acc[sb][:], [bacc[sb]], [bw_])
                if c_hi + 1 < 128:
                    P.memset("pool", w_[:, c_hi + 1:128], -BIG, [bw_])
                P.memset("pool", w_[0:64, c_hi:c_hi + 1], -BIG, [bw_])
                if c_lo - 1 >= 0:
                    P.memset("pool", w_[0:64, c_lo - 1:c_lo], BIG, [bw_])
                P.memset("pool", w_[0:64, c_lo:c_lo + 1], 2.0 * BIG, [bw_])
                P.memset("pool", w_[64:128, c_lo:c_lo + 1], BIG, [bw_])
                P.memset("pool", w_[64:128, c_hi:c_hi + 1], 2.0 * BIG, [bw_])
                P.memset("pool", w_[:, 0:1], 3.0 * BIG, [bw_])
                P.op("dve", lambda e, w_=w_: e.max(mx1[:], w_[:]), [bw_], [bmx1])
                P.op("dve", lambda e, w_=w_: e.match_replace(w2[:], mx1[:], w_[:], -BIG), [bw_, bmx1], [bw2])
                P.op("dve", lambda e: e.max(mx2[:], w2[:]), [bw2], [bmx2])
                g_, bg_ = ngm[sb % 2], bngm[sb % 2]
                P.ts("dve", g_[:], w_[:], mx2[:, 7:8], NEG, ALU.is_lt, ALU.mult, [bw_, bmx2], [bg_])
                P.tr(A.pst[:, 512 + (sb % 2) * 128:512 + (sb % 2) * 128 + 128], g_[:], A.ident[:], [bg_, A.bconst], [A.bpst[1]])
                P.copy("act", nst[nsl][:, sb * 128:(sb + 1) * 128], A.pst[:, 512 + (sb % 2) * 128:512 + (sb % 2) * 128 + 128],
                       [A.bpst[1]], [bnst[nsl]])
            P.dma("pool", D_["negmT"][:, Q * 512:(Q + 1) * 512], nst[nsl][:], [bnst[nsl]], [B_["negmT"]], snst[nsl])
        P.es = es_saved
    P.barrier()
    phase_A_nsa_2b(P, nc, io, A, D_, B_)


def phase_A_nsa_2b(P, nc, io, A, D_, B_):
    with ExitStack() as es3:
        P.es, es_saved = es3, P.es
        wk = attn_work(P)

        def load_fm(name):
            t = P.sb([128, S], BF16)
            b = Buf()
            sm = P.sem()
            for q4 in range(4):
                cs_ = slice(q4 * 2048, (q4 + 1) * 2048)
                P.dma("sp", t[:, cs_], D_[name][:, cs_], (), [b], sm)
            return t, b

        def load_tm(name):
            t = P.sb([128, 64, 128], BF16)
            b = Buf()
            sm = P.sem()
            for q4 in range(4):
                P.dma("sp", t[:, q4 * 16:(q4 + 1) * 16, :],
                      D_[name][q4 * 2048:(q4 + 1) * 2048, :].rearrange("(s p) d -> p s d", p=128), (), [b], sm)
            return t, b
        ks, bks = load_fm("ks")
        kw, bkw = load_fm("kw")
        vs, bvs = load_tm("vs")
        vw, bvw = load_tm("vw")
        ngT, bngT = load_fm("negmT")
        gsel = P.sb([128, S], BF16)
        bgsel = Buf()
        sgs = P.sem()
        for q4 in range(4):
            cs_ = slice(q4 * 2048, (q4 + 1) * 2048)
            P.dma("sp", gsel[:, cs_], io["gsel"][:, cs_], (), [bgsel], sgs)
        ntl = P.sb([128, 128], BF16)
        gs6 = P.sb([12, 6, 128], BF16)
        bc3 = Buf()
        sc3 = P.sem()
        P.dma("sp", ntl[:], io["negtri_lo"], (), [bc3], sc3)
        P.dma("sp", gs6[:], io["gsel6"], (), [bc3], sc3)
        lhsA = P.sb([9, 2, 128], BF16)
        rhsA = P.sb([9, 2, 512], BF16)
        btab = P.sb([128, 2, 1024], F32)
        bta = Buf()
        sta = P.sem()
        P.dma("sp", lhsA[:], io["lhsA"], (), [bta], sta)
        P.dma("sp", rhsA[:], io["rhsA"], (), [bta], sta)
        P.dma("sp", btab[:], io["btab"], (), [bta], sta)
        sg = [P.sb([12, 512], BF16) for _ in range(2)]
        bsg = [Buf() for _ in range(2)]
        ssg = [P.sem() for _ in range(2)]
        oc = [P.sb([128, 512], BF16) for _ in range(2)]
        boc = [Buf() for _ in range(2)]
        soc = [P.sem() for _ in range(2)]
        ra = P.sb([128, 512], F32)
        rb = P.sb([128, 512], F32)
        rc_ = P.sb([128, 512], F32)
        bra, brb, brc_ = Buf(), Buf(), Buf()
        psS = [(A.psb[0], A.bps[0]), (A.psb[1], A.bps[1])]
        psOs, bpsOs = A.psb[2], A.bps[2]
        psDs, bpsDs = A.psb[3], A.bps[3]
        psOw, bpsOw = A.psb[4], A.bps[4]
        psDw, bpsDw = A.psb[5], A.bps[5]
        psG, bpsG = A.psb[6], A.bps[6]
        ti = [0]

        def run_pass(tiles, score_fn, V, bV, po, bpo, pd, bpd, h, Q):
            n = len(tiles)
            pend = None
            for j, (kt, c0, c1) in enumerate(tiles):
                ps, bps = psS[ti[0] % 2]
                pt, bpt = wk["pt"][ti[0] % 3], wk["bpt"][ti[0] % 3]
                ti[0] += 1
                score_fn(ps, bps, kt, c0, c1)
                bidx = Q * 64 + kt
                P.act(pt[:, c0:c1], ps[:, c0:c1], AF.Exp, [bps, bta], [bpt], bias=btab[:, h, bidx:bidx + 1])
                if pend is not None:
                    pend()

                def pv(kt=kt, c0=c0, c1=c1, pt=pt, bpt=bpt, j=j):
                    P.mm(po[:, c0:c1], V[:, kt, :], pt[:, c0:c1], j == 0, j == n - 1, [bV, bpt], [bpo], sgc=True)
                    P.mm(pd[:, c0:c1], A.ones[:], pt[:, c0:c1], j == 0, j == n - 1, [A.bones, bpt], [bpd], sgc=True)
                pend = pv
            pend()

        for h in range(2):
            qn, bq = load_fm("q%d" % io["own_p"][h])
            for Q in range(S // 512):
                i2 = Q % 2
                P.dma("sp", sg[i2][0:6, :], D_["sgh"][:, Q * 512:(Q + 1) * 512], (), [bsg[i2]], ssg[i2])
                P.dma("sp", sg[i2][6:12, :], D_["sgl"][:, Q * 512:(Q + 1) * 512], (), [bsg[i2]], ssg[i2])
                P.dma("sp", oc[i2][:], D_["oc%d" % h][:, Q * 512:(Q + 1) * 512], (), [boc[i2]], soc[i2])

                def score_sel(ps, bps, kt, c0, c1, Q=Q):
                    q0 = Q * 512
                    diag = kt >= 4 * Q
                    P.mm(ps[:, c0:c1], ks[:, kt * 128:(kt + 1) * 128], qn[:, q0 + c0:q0 + c1], True, False, [bks, bq], [bps])
                    P.mm(ps[:, c0:c1], lhsA[:, h, :], rhsA[:, h, c0:c1], False, False, [bta], [bps])
                    P.mm(ps[:, c0:c1], gsel[:, kt * 128:(kt + 1) * 128], ngT[:, q0 + c0:q0 + c1], False, not diag,
                         [bgsel, bngT], [bps])
                    if diag:
                        P.mm(ps[:, c0:c0 + 128], A.ident[:], A.negtri[:], False, True, [A.bconst], [bps])

                def score_win(ps, bps, kt, c0, c1, Q=Q):
                    q0 = Q * 512
                    diag = kt >= 4 * Q
                    P.mm(ps[:, c0:c1], kw[:, kt * 128:(kt + 1) * 128], qn[:, q0 + c0:q0 + c1], True, False, [bkw, bq], [bps])
                    P.mm(ps[:, c0:c1], lhsA[:, h, :], rhsA[:, h, c0:c1], False, False, [bta], [bps])
                    if diag:
                        P.mm(ps[:, c0:c0 + 128], A.ident[:], A.negtri[:], False, True, [A.bconst], [bps])
                    else:
                        P.mm(ps[:, c1 - 128:c1], A.ident[:], ntl[:], False, True, [A.bconst, bc3], [bps])

                sel_tiles = [(kt, 0, 512) for kt in range(4 * Q)] + [(4 * Q + i, 128 * i, 512) for i in range(4)]
                win_tiles = []
                if Q >= 1:
                    win_tiles += [(4 * Q - 4 + i, 0, 128 * (i + 1)) for i in range(4)]
                win_tiles += [(4 * Q + i, 128 * i, 512) for i in range(4)]
                run_pass(win_tiles, score_win, vw, bvw, psOw, bpsOw, psDw, bpsDw, h, Q)
                run_pass(sel_tiles, score_sel, vs, bvs, psOs, bpsOs, psDs, bpsDs, h, Q)
                gt, bgt = wk["gt"][i2], wk["bgt"][i2]
                ot, bot = wk["ot"][i2], wk["bot"][i2]
                P.dma("sp", gt[:], D_["g%d" % h][:, Q * 512:(Q + 1) * 512], (), [bgt], wk["sg"][i2])
                P.mm(psG[:], gs6[:, 3 * h + 2, :], sg[i2][:], True, True, [bc3, bsg[i2]], [bpsG])
                P.ts("dve", ra[:], psDw[:], 1e-30, None, ALU.max, None, [bpsDw], [bra])
                P.recip(ra[:], ra[:], [bra], [bra])
                P.tt("dve", ra[:], ra[:], psG[:], ALU.mult, [bra, bpsG], [bra])
                P.tt("dve", rb[:], psOw[:], ra[:], ALU.mult, [bpsOw, bra], [brb])
                P.mm(psG[:], gs6[:, 3 * h + 1, :], sg[i2][:], True, True, [bc3, bsg[i2]], [bpsG])
                P.ts("dve", ra[:], psDs[:], 1e-30, None, ALU.max, None, [bpsDs], [bra])
                P.recip(ra[:], ra[:], [bra], [bra])
                P.tt("dve", ra[:], ra[:], psG[:], ALU.mult, [bra, bpsG], [bra])
                P.tt("dve", rc_[:], psOs[:], ra[:], ALU.mult, [bpsOs, bra], [brc_])
                P.tt("dve", rb[:], rb[:], rc_[:], ALU.add, [brb, brc_], [brb])
                P.mm(psG[:], gs6[:, 3 * h + 0, :], sg[i2][:], True, True, [bc3, bsg[i2]], [bpsG])
                P.tt("dve", rc_[:], oc[i2][:], psG[:], ALU.mult, [boc[i2], bpsG], [brc_])
                P.tt("dve", rb[:], rb[:], rc_[:], ALU.add, [brb, brc_], [brb])
                P.tt("dve", ot[:], rb[:], gt[:], ALU.mult, [brb, bgt], [bot])
                P.dma("pool", io["ogT"][h * 128:(h + 1) * 128, Q * 512:(Q + 1) * 512], ot[:], [bot], (), wk["so"][i2],
                      is_output=True)
        P.es = es_saved


def build_A_nsa(own_p=(0, 1)):
    nc = new_nc()
    io = {"own_p": list(own_p)}
    std_inputs(nc, io, [("hT", [D, S], BF16), ("w_in", [D, 1542], F32), ("ident", [128, 128], BF16),
                        ("negtri", [128, 128], BF16), ("g_qk", [128, 4], F32),
                        ("w_cmp_k", [4096, 128], F32), ("w_cmp_v", [4096, 128], F32), ("posT", [128, 32], F32),
                        ("cmap", [128, 4, 129], BF16), ("maskc", [128, 5, 512], BF16),
                        ("lhsC", [18, 4, 4, 128], BF16), ("rhsC", [4, 18, S], BF16),
                        ("gsel", [128, S], BF16), ("negtri_lo", [128, 128], BF16), ("gsel6", [12, 6, 128], BF16),
                        ("lhsA", [9, 2, 128], BF16), ("rhsA", [9, 2, 512], BF16), ("btab", [128, 2, 1024], F32)])
    io["ogT"] = nc.dram_tensor("ogT", [256, S], BF16, kind="ExternalOutput").ap()
    with ExitStack() as es:
        P = Prog(nc, es)
        phase_A_nsa(P, nc, io)
        P.emit()
    return nc


def maps_A_nsa(inputs, hT, C, cores):
    if "maskc" not in C:
        C.update(nsa_const_tables())
    w = inputs["w_in_d"][0]
    maps = []
    for c in cores:
        g = c // 2
        hd0 = 2 * c
        heads = [hd0, hd0 + 1] + [hh for hh in range(4 * g, 4 * g + 4) if hh not in (hd0, hd0 + 1)]
        cols = [w[:, 128 * hh:128 * (hh + 1)] for hh in heads]
        for base in (2048, 2560, 3072, 3584, 4096, 4608):
            cols.append(w[:, base + 128 * g:base + 128 * (g + 1)])
        cols.append(w[:, 5168 + 128 * hd0:5168 + 128 * (hd0 + 2)])
        cols.append(w[:, 5120 + 3 * hd0:5120 + 3 * (hd0 + 2)])
        m = {"hT": hT, "w_in": np.ascontiguousarray(np.concatenate(cols, axis=1))}
        for k in ("ident", "negtri", "cmap", "maskc", "gsel", "negtri_lo", "gsel6"):
            m[k] = C[k]
        kn = inputs["k_norm_d"][0]
        m["g_qk"] = np.ascontiguousarray(np.stack([inputs["q_norm_d"][0], kn[0], kn[1], kn[2]], axis=1).astype(np.float32))
        m["w_cmp_k"] = inputs["w_cmp_k_d"][0]
        m["w_cmp_v"] = inputs["w_cmp_v_d"][0]
        m["posT"] = np.ascontiguousarray(inputs["cmp_pos_d"][0].T)
        tabs = [nsa_cmp_tables(hh) for hh in heads]
        m["lhsC"] = np.ascontiguousarray(np.stack([t[0] for t in tabs], axis=1))
        m["rhsC"] = np.ascontiguousarray(np.stack([t[1] for t in tabs], axis=0))
        ht = [nsa_head_tables(hd0 + h) for h in range(2)]
        m["lhsA"] = np.ascontiguousarray(np.stack([t[0] for t in ht], axis=1))
        m["rhsA"] = np.ascontiguousarray(np.stack([t[1] for t in ht], axis=1))
        m["btab"] = np.ascontiguousarray(np.stack([t[2] for t in ht], axis=1))
        maps.append(m)
    return maps


_PROGS = {}
_CONST = {}


def _prog(key, fn):
    if key not in _PROGS:
        _PROGS[key] = fn()
    return _PROGS[key]


def _run(nc, maps):
    res = run_bass_kernel_spmd(nc, maps, core_ids=list(range(NCORE)))
    return res.results


def kernel(**inputs):
    inputs = {k: np.asarray(v) for k, v in inputs.items()}
    if not _CONST:
        _CONST.update(const_tables())
        sb_consts(_CONST)
        _CONST.update(nsa_const_tables())
    C = _CONST
    cores = list(range(NCORE))
    x = inputs["x"][0]
    xT = np.ascontiguousarray(x.T)
    xs = [np.ascontiguousarray(xT[:, c * TOK:(c + 1) * TOK]) for c in cores]
    norms = [inputs["norm_a"][0], inputs["norm_b"][0], inputs["norm_c"][0], inputs["norm_d"][0]]
    wouts = [inputs["w_out_a"][0], inputs["w_out_b"][0], inputs["w_out_c"][0], inputs["w_out_d"][0]]
    r = _run(_prog("N", lambda: build_B(False, True)), [{"xT": xs[c], "g": col(norms[0], 16)} for c in cores])
    hT = np.ascontiguousarray(np.concatenate([r[c]["hT"] for c in cores], axis=1))
    builders = [("A0", build_A_mla, maps_A_mla), ("A1", build_A_moba, maps_A_moba),
                ("A2", build_A_sb, maps_A_sb), ("A3", build_A_nsa, maps_A_nsa)]
    for l in range(4):
        key, bfn, mfn = builders[l]
        r = _run(_prog(key, bfn), mfn(inputs, hT, C, cores))
        ogT = np.concatenate([r[c]["ogT"] for c in cores], axis=0)
        last = (l == 3)
        maps = []
        for c in cores:
            m = {"xT": xs[c], "ogT": np.ascontiguousarray(ogT[:, c * TOK:(c + 1) * TOK]), "w": wouts[l]}
            if not last:
                m["g"] = col(norms[l + 1], 16)
            maps.append(m)
        r = _run(_prog("B%d" % int(last), lambda: build_B(True, not last)), maps)
        xs = [r[c]["xo"] for c in cores]
        if not last:
            hT = np.ascontiguousarray(np.concatenate([r[c]["hT"] for c in cores], axis=1))
    out = np.concatenate(xs, axis=1)
    return np.ascontiguousarray(out.T)[None].astype(np.float32)
```
